# Optimizing a Trainium2 kernel written in Bass

```python
import math
import jax, jax.numpy as jnp
from jax import lax
import numpy as np

D_MODEL = 1024
BATCH = 8
SEQ = 4096
DEPTH = 2

GRID_W = 64
Q_BLOCK = 128
EPS = 1e-6
ROPE_THETA = 10000.0

A_HEADS = 8
A_Q_LORA = 256
A_KV_LORA = 128
A_NOPE = 64
A_ROPE = 32
A_V = 64
B_HEADS = 8
B_KV_HEADS = 2
B_HEAD_DIM = 64
C_HEADS = 16
C_KV_HEADS = 4
C_HEAD_DIM = 64
C_WINDOW = 128
REL_BUCKETS = 32
REL_MAX_DIST = 128
N_GROUPS = 4
EXPERTS_PER_GROUP = 4
N_EXPERTS = N_GROUPS * EXPERTS_PER_GROUP
TOP_K_IN_GROUP = 2
D_EXPERT = 512

AB_PARTS = (A_Q_LORA, A_KV_LORA, A_ROPE, B_HEADS * B_HEAD_DIM,
            B_KV_HEADS * B_HEAD_DIM, B_KV_HEADS * B_HEAD_DIM)
AB_IN = sum(AB_PARTS)
AB_SPLITS = tuple(int(v) for v in np.cumsum(AB_PARTS)[:-1])
AB_OUT = A_HEADS * A_V + B_HEADS * B_HEAD_DIM
C_PARTS = (C_HEADS * C_HEAD_DIM, C_KV_HEADS * C_HEAD_DIM, C_KV_HEADS * C_HEAD_DIM)
C_IN = sum(C_PARTS)
C_SPLITS = tuple(int(v) for v in np.cumsum(C_PARTS)[:-1])
C_OUT = C_HEADS * C_HEAD_DIM
N_EVEN = (DEPTH + 1) // 2
N_ODD = DEPTH // 2

kernel_name = "hybrid_mla_axialgqa_swa_hmoe_encoder"


def rms_norm(x, gain):
    xf = x.astype(jnp.float32)
    y = xf * lax.rsqrt(jnp.mean(xf * xf, axis=-1, keepdims=True) + EPS)
    return (y * gain.astype(jnp.float32)).astype(x.dtype)


def rope_cos_sin(pos, dim):
    inv = ROPE_THETA ** (-jnp.arange(0, dim, 2, dtype=jnp.float32) / dim)
    ang = pos.astype(jnp.float32)[:, None] * inv[None, :]
    return jnp.cos(ang), jnp.sin(ang)


def apply_rope(x, cos, sin):
    x1, x2 = jnp.split(x, 2, axis=-1)
    cos = cos.astype(x.dtype)
    sin = sin.astype(x.dtype)
    return jnp.concatenate([x1 * cos - x2 * sin, x2 * cos + x1 * sin], axis=-1)


def to_blocks(t):
    b, s = t.shape[:2]
    t = t.reshape((b, s // Q_BLOCK, Q_BLOCK) + t.shape[2:])
    return jnp.moveaxis(t, 1, 0)


def from_blocks(t):
    t = jnp.moveaxis(t, 0, 1)
    return t.reshape((t.shape[0], t.shape[1] * t.shape[2]) + t.shape[3:])


def mla_mix(q_lat, kv_lat, k_rope, q_a_gain, w_q_up, kv_a_gain, w_kv_up,
            qn_gain, kn_gain, qr_gain, kr_gain):
    b, s, _ = q_lat.shape
    cos, sin = rope_cos_sin(jnp.arange(s), A_ROPE)
    q = (rms_norm(q_lat, q_a_gain) @ w_q_up).reshape(b, s, A_HEADS, A_NOPE + A_ROPE)
    kv = (rms_norm(kv_lat, kv_a_gain) @ w_kv_up).reshape(b, s, A_HEADS, A_NOPE + A_V)
    q_nope = rms_norm(q[..., :A_NOPE], qn_gain)
    q_rot = apply_rope(rms_norm(q[..., A_NOPE:], qr_gain), cos[:, None], sin[:, None])
    k_nope = rms_norm(kv[..., :A_NOPE], kn_gain)
    v = kv[..., A_NOPE:]
    k_rot = apply_rope(rms_norm(k_rope, kr_gain), cos, sin)
    scale = (A_NOPE + A_ROPE) ** -0.5

    def block(args):
        qn, qr = args
        sc = (jnp.einsum('bqhd,bkhd->bhqk', qn, k_nope)
              + jnp.einsum('bqhr,bkr->bhqk', qr, k_rot))
        p = jax.nn.softmax(sc.astype(jnp.float32) * scale, axis=-1).astype(v.dtype)
        return jnp.einsum('bhqk,bkhd->bqhd', p, v)

    o = lax.map(block, (to_blocks(q_nope), to_blocks(q_rot)))
    return from_blocks(o).reshape(b, s, A_HEADS * A_V)


def axial_gqa_mix(q, k, v, q_gain, k_gain):
    b, s = q.shape[:2]
    rows = s // GRID_W
    row_pos = jnp.repeat(jnp.arange(rows), GRID_W)
    col_pos = jnp.tile(jnp.arange(GRID_W), rows)
    half = B_HEAD_DIM // 2
    cos_r, sin_r = rope_cos_sin(row_pos, half)
    cos_c, sin_c = rope_cos_sin(col_pos, half)

    def axial(t):
        return jnp.concatenate(
            [apply_rope(t[..., :half], cos_r[:, None], sin_r[:, None]),
             apply_rope(t[..., half:], cos_c[:, None], sin_c[:, None])], axis=-1)

    grp = B_HEADS // B_KV_HEADS
    qn = axial(rms_norm(q, q_gain)).reshape(b, s, B_KV_HEADS, grp, B_HEAD_DIM)
    kn = axial(rms_norm(k, k_gain))
    scale = B_HEAD_DIM ** -0.5

    def block(qb):
        sc = jnp.einsum('bqhgd,bkhd->bhgqk', qb, kn)
        p = jax.nn.softmax(sc.astype(jnp.float32) * scale, axis=-1).astype(v.dtype)
        return jnp.einsum('bhgqk,bkhd->bqhgd', p, v)

    o = lax.map(block, to_blocks(qn))
    return from_blocks(o).reshape(b, s, B_HEADS * B_HEAD_DIM)


def t5_bucket(rel):
    nb = REL_BUCKETS // 2
    max_exact = nb // 2
    ret = jnp.where(rel > 0, nb, 0)
    n = jnp.abs(rel)
    nf = jnp.maximum(n, 1).astype(jnp.float32)
    large = max_exact + (jnp.log(nf / max_exact) / math.log(REL_MAX_DIST / max_exact)
                         * (nb - max_exact)).astype(jnp.int32)
    large = jnp.minimum(large, nb - 1)
    return ret + jnp.where(n < max_exact, n, large)


def window_gqa_mix(q, k, v, q_gain, k_gain, sink, rel_table):
    b, s = q.shape[:2]
    grp = C_HEADS // C_KV_HEADS
    span = Q_BLOCK + 2 * C_WINDOW
    qn = rms_norm(q, q_gain).reshape(b, s, C_KV_HEADS, grp, C_HEAD_DIM)
    kn = rms_norm(k, k_gain)
    pad = ((0, 0), (C_WINDOW, C_WINDOW), (0, 0), (0, 0))
    kp = jnp.pad(kn, pad)
    vp = jnp.pad(v, pad)
    qi = jnp.arange(Q_BLOCK)[:, None]
    kj = jnp.arange(span)[None, :]
    rel = kj - C_WINDOW - qi
    in_band = jnp.abs(rel) <= C_WINDOW
    bias = rel_table[t5_bucket(rel)]
    bias = jnp.transpose(bias, (2, 0, 1)).reshape(C_KV_HEADS, grp, Q_BLOCK, span).astype(jnp.float32)
    sink_f = sink.astype(jnp.float32).reshape(C_KV_HEADS, grp, 1, 1)
    scale = C_HEAD_DIM ** -0.5

    def block(args):
        i, qb = args
        start = i * Q_BLOCK
        kb = lax.dynamic_slice_in_dim(kp, start, span, axis=1)
        vb = lax.dynamic_slice_in_dim(vp, start, span, axis=1)
        key_pos = start - C_WINDOW + kj
        mask = in_band & (key_pos >= 0) & (key_pos < s)
        sc = jnp.einsum('bqhgd,bkhd->bhgqk', qb, kb).astype(jnp.float32) * scale + bias
        sc = jnp.where(mask, sc, -jnp.inf)
        sink_col = jnp.broadcast_to(sink_f, sc.shape[:-1] + (1,))
        p = jax.nn.softmax(jnp.concatenate([sc, sink_col], axis=-1), axis=-1)[..., :span]
        return jnp.einsum('bhgqk,bkhd->bqhgd', p.astype(vb.dtype), vb)

    o = lax.map(block, (jnp.arange(s // Q_BLOCK), to_blocks(qn)))
    return from_blocks(o).reshape(b, s, C_HEADS * C_HEAD_DIM)


def hier_moe(h, w_group, b_group, w_router, b_router, w_gate, w_up, w_down):
    b, s, d = h.shape
    xt = h.reshape(b * s, d)
    g_prob = jax.nn.softmax((xt @ w_group).astype(jnp.float32) + b_group.astype(jnp.float32), axis=-1)
    g_p, g_idx = lax.top_k(g_prob, 1)
    e_logits = ((xt @ w_router).astype(jnp.float32) + b_router.astype(jnp.float32)
                ).reshape(-1, N_GROUPS, EXPERTS_PER_GROUP)
    e_logits = jnp.take_along_axis(e_logits, g_idx[:, :, None], axis=1)[:, 0]
    e_prob = jax.nn.softmax(e_logits, axis=-1)
    e_p, e_loc = lax.top_k(e_prob, TOP_K_IN_GROUP)
    wts = g_p * e_p / jnp.sum(e_p, axis=-1, keepdims=True)
    e_idx = g_idx * EXPERTS_PER_GROUP + e_loc
    gates = jnp.sum(jax.nn.one_hot(e_idx, N_EXPERTS, dtype=jnp.float32) * wts[..., None], axis=1)
    gates = gates.astype(xt.dtype)
    out = jnp.zeros_like(xt)
    for e in range(N_EXPERTS):
        hid = jax.nn.silu(xt @ w_gate[e]) * (xt @ w_up[e])
        out = out + gates[:, e:e + 1] * (hid @ w_down[e])
    return out.reshape(b, s, d)


def setup_inputs(seed: int = 0) -> dict:
    key = jax.random.key(seed)
    ks = iter(jax.random.split(key, 40))

    def dense(shape, fan_in):
        return jax.random.normal(next(ks), shape, jnp.float32) * (fan_in ** -0.5)

    def gain(shape):
        return 1.0 + 0.02 * jax.random.normal(next(ks), shape, jnp.float32)

    def small(shape, scale):
        return scale * jax.random.normal(next(ks), shape, jnp.float32)

    return {
        "x": jax.random.normal(next(ks), (BATCH, SEQ, D_MODEL), jnp.float32),
        "mix_norm": gain((DEPTH, D_MODEL)),
        "ffn_norm": gain((DEPTH, D_MODEL)),
        "w_in_ab": dense((N_EVEN, D_MODEL, AB_IN), D_MODEL),
        "mla_q_a_norm": gain((N_EVEN, A_Q_LORA)),
        "mla_w_q_up": dense((N_EVEN, A_Q_LORA, A_HEADS * (A_NOPE + A_ROPE)), A_Q_LORA),
        "mla_kv_a_norm": gain((N_EVEN, A_KV_LORA)),
        "mla_w_kv_up": dense((N_EVEN, A_KV_LORA, A_HEADS * (A_NOPE + A_V)), A_KV_LORA),
        "mla_qn_gain": gain((N_EVEN, A_NOPE)),
        "mla_kn_gain": gain((N_EVEN, A_NOPE)),
        "mla_qr_gain": gain((N_EVEN, A_ROPE)),
        "mla_kr_gain": gain((N_EVEN, A_ROPE)),
        "gqa_q_gain": gain((N_EVEN, B_HEAD_DIM)),
        "gqa_k_gain": gain((N_EVEN, B_HEAD_DIM)),
        "w_out_ab": dense((N_EVEN, AB_OUT, D_MODEL), AB_OUT),
        "w_in_c": dense((N_ODD, D_MODEL, C_IN), D_MODEL),
        "win_q_gain": gain((N_ODD, C_HEAD_DIM)),
        "win_k_gain": gain((N_ODD, C_HEAD_DIM)),
        "win_sink": small((N_ODD, C_HEADS), 0.5),
        "w_out_c": dense((N_ODD, C_OUT, D_MODEL), C_OUT),
        "rel_bias": small((REL_BUCKETS, C_HEADS), 0.5),
        "moe_w_group": dense((DEPTH, D_MODEL, N_GROUPS), D_MODEL),
        "moe_b_group": small((DEPTH, N_GROUPS), 0.01),
        "moe_w_router": dense((DEPTH, D_MODEL, N_EXPERTS), D_MODEL),
        "moe_b_router": small((DEPTH, N_EXPERTS), 0.01),
        "moe_w_gate": dense((DEPTH, N_EXPERTS, D_MODEL, D_EXPERT), D_MODEL),
        "moe_w_up": dense((DEPTH, N_EXPERTS, D_MODEL, D_EXPERT), D_MODEL),
        "moe_w_down": dense((DEPTH, N_EXPERTS, D_EXPERT, D_MODEL), D_EXPERT),
    }


def reference(x, mix_norm, ffn_norm, w_in_ab, mla_q_a_norm, mla_w_q_up, mla_kv_a_norm,
              mla_w_kv_up, mla_qn_gain, mla_kn_gain, mla_qr_gain, mla_kr_gain,
              gqa_q_gain, gqa_k_gain, w_out_ab, w_in_c, win_q_gain, win_k_gain,
              win_sink, w_out_c, rel_bias, moe_w_group, moe_b_group, moe_w_router,
              moe_b_router, moe_w_gate, moe_w_up, moe_w_down):
    b, s, _ = x.shape
    for layer in range(DEPTH):
        i = layer // 2
        h = rms_norm(x, mix_norm[layer])
        if layer % 2 == 0:
            proj = h @ w_in_ab[i]
            q_lat, kv_lat, k_rope, bq, bk, bv = jnp.split(proj, AB_SPLITS, axis=-1)
            out_a = mla_mix(q_lat, kv_lat, k_rope, mla_q_a_norm[i], mla_w_q_up[i],
                            mla_kv_a_norm[i], mla_w_kv_up[i], mla_qn_gain[i],
                            mla_kn_gain[i], mla_qr_gain[i], mla_kr_gain[i])
            out_b = axial_gqa_mix(bq.reshape(b, s, B_HEADS, B_HEAD_DIM),
                                  bk.reshape(b, s, B_KV_HEADS, B_HEAD_DIM),
                                  bv.reshape(b, s, B_KV_HEADS, B_HEAD_DIM),
                                  gqa_q_gain[i], gqa_k_gain[i])
            x = x + jnp.concatenate([out_a, out_b], axis=-1) @ w_out_ab[i]
        else:
            proj = h @ w_in_c[i]
            cq, ck, cv = jnp.split(proj, C_SPLITS, axis=-1)
            out_c = window_gqa_mix(cq.reshape(b, s, C_HEADS, C_HEAD_DIM),
                                   ck.reshape(b, s, C_KV_HEADS, C_HEAD_DIM),
                                   cv.reshape(b, s, C_KV_HEADS, C_HEAD_DIM),
                                   win_q_gain[i], win_k_gain[i], win_sink[i], rel_bias)
            x = x + out_c @ w_out_c[i]
        x = x + hier_moe(rms_norm(x, ffn_norm[layer]), moe_w_group[layer], moe_b_group[layer],
                         moe_w_router[layer], moe_b_router[layer], moe_w_gate[layer],
                         moe_w_up[layer], moe_w_down[layer])
    return x
```

```python
from contextlib import ExitStack
import math
import numpy as np
import concourse.bass as bass
import concourse.mybir as mybir
from concourse.bass_utils import run_bass_kernel_spmd

F32 = mybir.dt.float32
BF16 = mybir.dt.bfloat16
ALU = mybir.AluOpType
AF = mybir.ActivationFunctionType
AX = mybir.AxisListType

ENGS = ("pe", "act", "dve", "pool", "sp")
S = 4096
D = 1024
NT = S // 128
EPS = 1e-6
NEG = -1.0e30
STRICT_SAME_ENGINE = True


class _Op:
    __slots__ = ("fn", "waits", "signal", "dma")

    def __init__(self, fn, waits, dma):
        self.fn = fn
        self.waits = waits
        self.signal = False
        self.dma = dma


class Prog:
    def __init__(self, n_dma_sems=32):
        self.ops = {e: [] for e in ENGS}
        self.done = {e: 0 for e in ENGS}
        self.sigbase = {e: 0 for e in ENGS}
        self.sigmap = {e: {} for e in ENGS}
        self.seen = {e: {} for e in ENGS}
        self.res = {}
        self.n_dma = n_dma_sems
        self.dma_cnt = [0] * n_dma_sems
        self.dma_rr = 0
        self.n_sw = 8
        self.sw_rr = 0

    def _deps(self, eng, reads, writes):
        deps = {}

        def add(p, kind):
            if p is None:
                return
            prod, tick = p
            if prod == eng and kind != "raw" and not STRICT_SAME_ENGINE:
                return
            if prod == "pe" and eng == "pe":
                return
            if tick > deps.get(prod, -1):
                deps[prod] = tick

        for k in reads:
            r = self.res.get(k)
            if r is not None:
                add(r[0], "raw")
        for k in writes:
            r = self.res.get(k)
            if r is not None:
                add(r[0], "waw")
                for rd in r[1]:
                    add(rd, "war")
        waits = []
        seen = self.seen[eng]
        for prod, tick in deps.items():
            if seen.get(prod, -1) >= tick:
                continue
            seen[prod] = tick
            waits.append((prod, tick))
            if not isinstance(prod, tuple):
                assert tick >= self.done[prod], "dependency on flushed op without barrier"
                self.ops[prod][tick - self.done[prod]].signal = True
        return waits

    def _record(self, me, reads, writes):
        for k in reads:
            r = self.res.get(k)
            if r is None:
                self.res[k] = [None, [me]]
            else:
                r[1].append(me)
        for k in writes:
            self.res[k] = [me, []]

    @staticmethod
    def _excl(reads, writes):
        ps = [k for k in reads if (k if isinstance(k, str) else k[0]).startswith("ps_")]
        if ps:
            writes = list(writes) + [k for k in ps if k not in writes]
        return reads, writes

    _cap = None
    _ns = None
    _atom = None
    SHARED_STR = ("ps_", "a_", "c_", "m_")
    SHARED_NAMES = {"gqk", "invA", "invB", "Wq", "Wkv", "Wab", "Wc", "QT", "KT", "Vd", "ps_pp", "ps_pq", "ps_pkv",
                    "QTs", "KTs", "BTs", "pr", "tab", "xt", "xn", "xnT", "junk", "ssx", "rx", "rx_t",
                    "acc", "hT", "Lall", "gates"}

    def _nskey(self, k):
        if self._ns is None:
            return k
        if isinstance(k, str):
            if k.startswith(self.SHARED_STR) or k in self.SHARED_NAMES:
                return k
            return (k, "ns", self._ns)
        if k[0] in self.SHARED_NAMES or k[0].startswith("ps_"):
            return k
        return tuple(k) + ("ns", self._ns)

    def begin_capture(self, ns):
        self._cap = []
        self._ns = ns
        self._atom = None

    def atom_begin(self):
        if self._cap is not None:
            self._atom = []

    def atom_end(self):
        if self._cap is not None and self._atom is not None:
            self._cap.append(self._atom)
            self._atom = None

    def _capture(self, item):
        if self._atom is not None:
            self._atom.append(item)
        else:
            self._cap.append([item])

    def end_capture(self):
        c = self._cap
        self._cap = None
        self._ns = None
        return c

    def replay(self, streams):
        idx = [0] * len(streams)
        live = True
        while live:
            live = False
            for i, st in enumerate(streams):
                if idx[i] < len(st):
                    unit = st[idx[i]]
                    idx[i] += 1
                    live = True
                    for kind, eng, fn, r, w in unit:
                        (self.op if kind == "op" else self.dma)(eng, fn, r, w)

    def op(self, eng, fn, reads=(), writes=()):
        if self._cap is not None:
            self._capture(("op", eng, fn, [self._nskey(k) for k in reads], [self._nskey(k) for k in writes]))
            return
        reads, writes = self._excl(reads, writes)
        waits = self._deps(eng, reads, writes)
        idx = self.done[eng] + len(self.ops[eng])
        self.ops[eng].append(_Op(fn, waits, None))
        self._record((eng, idx), reads, writes)
        return idx

    def dma(self, eng, fn, reads=(), writes=()):
        if self._cap is not None:
            self._capture(("dma", eng, fn, [self._nskey(k) for k in reads], [self._nskey(k) for k in writes]))
            return
        waits = self._deps(eng, reads, writes)
        if eng == "pool":
            j = self.n_dma - self.n_sw + self.sw_rr
            self.sw_rr = (self.sw_rr + 1) % self.n_sw
        else:
            j = self.dma_rr
            self.dma_rr = (j + 1) % (self.n_dma - self.n_sw)
        prev = self.dma_cnt[j]
        prod = ("dma", j)
        seen = self.seen[eng]
        if prev > 0 and seen.get(prod, -1) < prev:
            seen[prod] = prev
            waits.append((prod, prev))
        self.dma_cnt[j] = prev + 1
        self.ops[eng].append(_Op(fn, waits, (j, prev + 1)))
        self._record((prod, prev + 1), reads, writes)

    def barrier(self):
        last = {}
        for e in ENGS:
            if self.ops[e]:
                for i in range(len(self.ops[e]) - 1, -1, -1):
                    o = self.ops[e][i]
                    if o.dma is None and o.fn is not None:
                        o.signal = True
                        last[e] = self.done[e] + i
                        break
        for f in ENGS:
            waits = []
            seen = self.seen[f]
            for e, tick in last.items():
                if e != f and seen.get(e, -1) < tick:
                    waits.append((e, tick))
                    seen[e] = tick
            for j in range(self.n_dma):
                c = self.dma_cnt[j]
                prod = ("dma", j)
                if c > 0 and seen.get(prod, -1) < c:
                    waits.append((prod, c))
                    seen[prod] = c
            self.ops[f].append(_Op(None, waits, None))
        for f in ENGS:
            for e in ENGS:
                n = self.done[e] + len(self.ops[e]) - 1
                if e != f:
                    if e in last:
                        self.seen[f][e] = max(self.seen[f].get(e, -1), last[e])
        self.res = {}

    def flush(self, block, esem, dsem):
        engobj = {"pe": "tensor", "act": "scalar", "dve": "vector",
                  "pool": "gpsimd", "sp": "sync"}
        for e in ENGS:
            c = self.sigbase[e]
            for i, o in enumerate(self.ops[e]):
                if o.signal:
                    c += 1
                    self.sigmap[e][self.done[e] + i] = c

        def run(e):
            def body(eng):
                for o in self.ops[e]:
                    for prod, tick in o.waits:
                        if isinstance(prod, tuple):
                            eng.wait_ge(dsem[prod[1]], 16 * tick)
                        else:
                            eng.wait_ge(esem[prod], self.sigmap[prod][tick])
                    if o.fn is None:
                        continue
                    ins = o.fn(eng)
                    if o.dma is not None:
                        ins.then_inc(dsem[o.dma[0]], 16)
                    elif o.signal:
                        ins.then_inc(esem[e], 1)
            return body

        for e in ENGS:
            if self.ops[e]:
                getattr(block, engobj[e])(run(e))
        for e in ENGS:
            self.sigbase[e] = max([self.sigbase[e]] + [v for v in self.sigmap[e].values()])
            self.done[e] += len(self.ops[e])
            self.ops[e] = []


def bc_rows(ap, nparts=128):
    n = ap.shape[-1]
    return bass.AP(ap.tensor, ap.offset, [[0, nparts], [1, n]])


class Ctx:
    def __getattr__(self, name):
        if name in W_SHAPES:
            ap = self.nc.dram_tensor(name, W_SHAPES[name], F32, kind="ExternalInput").ap()
            self.__dict__[name] = ap
            self.used_inputs.append(name)
            return ap
        raise AttributeError(name)

    def tap(self, name, src_ap, shape, reads):
        if name not in self.taps:
            return
        d = self.nc.dram_tensor("tap_" + name, list(shape), F32, kind="ExternalOutput").ap()
        self.P.dma("sp", lambda e: e.dma_start(out=d, in_=src_ap), reads=reads, writes=["tap_" + name])


def moe_phase(K, layer, x_in, x_out, n_exp=16, stage=9, plevel=9):
    nc, P = K.nc, K.P
    SBT = 16
    NSB = NT // SBT
    with ExitStack() as es:
        def sb(name, shape, dt):
            return es.enter_context(nc.sbuf_tensor(name + "_L%d" % layer, shape, dt))

        def ps(name, shape, dt):
            return es.enter_context(nc.psum_tensor(name + "_L%d" % layer, shape, dt))

        acc = sb("m_acc", [128, SBT, D], F32)
        hT = sb("m_hT", [128, 8, SBT * 128], BF16)
        wgu = [sb("m_wgu%d" % i, [128, 8, 1024], BF16) for i in range(2)]
        wd = [sb("m_wd%d" % i, [128, 4, 1024], BF16) for i in range(2)]
        hid = [sb("m_hid%d" % i, [128, 4, 512], BF16) for i in range(2)]
        sg = [sb("m_sg%d" % i, [128, 512], F32) for i in range(2)]
        junks = [sb("m_junk%d" % i, [128, D], BF16) for i in range(2)]
        h32s = [sb("m_h32%d" % i, [128, D], F32) for i in range(2)]
        hT32s = [sb("m_hT32%d" % i, [128, 8, 128], F32) for i in range(2)]
        gain = sb("m_gain", [128, D], F32)
        wr = sb("m_wr", [128, 8, 20], F32)
        rb = sb("m_rb", [128, 20], F32)
        sss = [sb("m_ss%d" % i, [128, 4], F32) for i in range(2)]
        Lall = sb("m_L", [128, SBT, 20], F32)
        gates = sb("m_gates", [128, SBT, 16], F32)
        r_gmax = sb("r_gmax", [128, SBT], F32)
        r_gsh = sb("r_gsh", [128, SBT, 4], F32)
        r_gexp = sb("r_gexp", [128, SBT, 4], F32)
        r_gsum = sb("r_gsum", [128, SBT], F32)
        r_gp = sb("r_gp", [128, SBT], F32)
        r_pen = sb("r_pen", [128, SBT, 4], F32)
        r_em = sb("r_em", [128, SBT, 16], F32)
        r_m1 = sb("r_m1", [128, SBT], F32)
        r_mask1 = sb("r_mask1", [128, SBT, 16], F32)
        r_em2 = sb("r_em2", [128, SBT, 16], F32)
        r_m2 = sb("r_m2", [128, SBT], F32)
        r_mask2 = sb("r_mask2", [128, SBT, 16], F32)
        r_ed = sb("r_ed", [128, SBT], F32)
        r_w1 = sb("r_w1", [128, SBT], F32)
        r_w2 = sb("r_w2", [128, SBT], F32)

        ptr = ps("m_ptr", [128, 8, 128], F32)
        pG = [ps("m_pG%d" % i, [128, 512], F32) for i in range(2)]
        pU = [ps("m_pU%d" % i, [128, 512], F32) for i in range(2)]
        pY = [ps("m_pY%d" % i, [128, 512], F32) for i in range(2)]

        P.dma("sp", lambda e: e.dma_start(out=gain[:], in_=bc_rows(K.ffn_norm[layer:layer + 1, :])),
              writes=["m_gain"])
        P.dma("sp", lambda e: e.dma_start(out=wr[:, :, 0:4],
                                          in_=K.moe_w_group[layer].rearrange("(c p) n -> p c n", p=128)),
              writes=["m_wr"])
        P.dma("sp", lambda e: e.dma_start(out=wr[:, :, 4:20],
                                          in_=K.moe_w_router[layer].rearrange("(c p) n -> p c n", p=128)),
              writes=["m_wr"])
        P.dma("sp", lambda e: e.dma_start(out=rb[:, 0:4], in_=bc_rows(K.moe_b_group[layer:layer + 1, :])),
              writes=["m_rb"])
        P.dma("sp", lambda e: e.dma_start(out=rb[:, 4:20], in_=bc_rows(K.moe_b_router[layer:layer + 1, :])),
              writes=["m_rb"])

        def load_w(e_idx, buf):
            wg_src = K.moe_w_gate[layer, e_idx].rearrange("(c p) n -> p c n", p=128)
            wu_src = K.moe_w_up[layer, e_idx].rearrange("(c p) n -> p c n", p=128)
            wd_src = K.moe_w_down[layer, e_idx].rearrange("(c p) n -> p c n", p=128)
            for h in range(2):
                P.dma("pool", lambda e, h=h: e.dma_start(out=wgu[buf][:, 4 * h:4 * h + 4, 0:512],
                                                       in_=wg_src[:, 4 * h:4 * h + 4, :]),
                      writes=[("wgu", buf, 0, h)])
                P.dma("pool", lambda e, h=h: e.dma_start(out=wgu[buf][:, 4 * h:4 * h + 4, 512:1024],
                                                       in_=wu_src[:, 4 * h:4 * h + 4, :]),
                      writes=[("wgu", buf, 1, h)])
            P.dma("pool", lambda e: e.dma_start(out=wd[buf][:], in_=wd_src), writes=[("wd", buf)])

        pend = [None]
        nblk = [0]

        def emit_gu(ex, buf, b, n):
            hb = hid[n % 2]
            for f in range(4):
                i2 = f % 2
                for c in range(8):
                    P.op("pe", lambda e, c=c, f=f, i2=i2: e.matmul(
                        pG[i2][:], lhsT=wgu[buf][:, c, f * 128:(f + 1) * 128],
                        rhs=hT[:, c, b * 512:(b + 1) * 512], start=(c == 0), stop=(c == 7)),
                        reads=[("wgu", buf, 0, c // 4), ("hT", b)], writes=[("ps_G", i2)])
                for c in range(8):
                    P.op("pe", lambda e, c=c, f=f, i2=i2: e.matmul(
                        pU[i2][:], lhsT=wgu[buf][:, c, 512 + f * 128:512 + (f + 1) * 128],
                        rhs=hT[:, c, b * 512:(b + 1) * 512], start=(c == 0), stop=(c == 7)),
                        reads=[("wgu", buf, 1, c // 4), ("hT", b)], writes=[("ps_U", i2)])
                P.op("act", lambda e, i2=i2: e.activation(out=sg[i2][:], in_=pG[i2][:], func=AF.Silu),
                     reads=[("ps_G", i2)], writes=[("sg", i2)])
                P.op("dve", lambda e, i2=i2, f=f: e.tensor_tensor(out=hb[:, f, :], in0=sg[i2][:], in1=pU[i2][:], op=ALU.mult),
                     reads=[("sg", i2), ("ps_U", i2)], writes=[("hid", n % 2, f)])

        def emit_y(ex, buf, b, n):
            hb = hid[n % 2]
            for s4 in range(4):
                tl = b * 4 + s4
                for nh in range(2):
                    yb = (s4 * 2 + nh) % 2
                    for f in range(4):
                        P.op("pe", lambda e, f=f, s4=s4, nh=nh, yb=yb: e.matmul(
                            pY[yb][:], lhsT=hb[:, f, s4 * 128:(s4 + 1) * 128],
                            rhs=wd[buf][:, f, nh * 512:(nh + 1) * 512], start=(f == 0), stop=(f == 3)),
                            reads=[("hid", n % 2, f), ("wd", buf)], writes=["ps_Y%d" % yb])
                    P.op("dve", lambda e, tl=tl, nh=nh, yb=yb: e.scalar_tensor_tensor(
                        out=acc[:, tl, nh * 512:(nh + 1) * 512], in0=pY[yb][:],
                        scalar=gates[:, tl, ex:ex + 1], in1=acc[:, tl, nh * 512:(nh + 1) * 512],
                        op0=ALU.mult, op1=ALU.add),
                        reads=["ps_Y%d" % yb, "gates", ("acc", tl)], writes=[("acc", tl)])

        for sbi in range(NSB):
            def prep_tile(tl):
                t = sbi * SBT + tl
                junk = junks[tl % 2]
                h32 = h32s[tl % 2]
                hT32 = hT32s[tl % 2]
                ss = sss[tl % 2]
                P.dma("sp", lambda e, t=t, tl=tl: e.dma_start(out=acc[:, tl, :], in_=x_in[t * 128:(t + 1) * 128, :]),
                      writes=[("acc", tl)])
                if plevel < 1:
                    return
                P.op("act", lambda e, tl=tl: e.activation(out=junk[:], in_=acc[:, tl, :], func=AF.Square,
                                                          accum_out=ss[:, 0:1]),
                     reads=[("acc", tl)], writes=["junk", "ss0"])
                P.op("dve", lambda e: e.tensor_scalar(out=ss[:, 1:2], in0=ss[:, 0:1], scalar1=1.0 / D, scalar2=EPS,
                                                      op0=ALU.mult, op1=ALU.add), reads=["ss0"], writes=["ss1"])
                P.op("act", lambda e: e.activation(out=ss[:, 2:3], in_=ss[:, 1:2], func=AF.Sqrt),
                     reads=["ss1"], writes=["ss2"])
                P.op("dve", lambda e: e.reciprocal(out=ss[:, 3:4], in_=ss[:, 2:3]), reads=["ss2"], writes=["ss3"])
                if plevel < 2:
                    return
                P.op("dve", lambda e, tl=tl: e.scalar_tensor_tensor(out=h32[:], in0=acc[:, tl, :], scalar=ss[:, 3:4],
                                                                     in1=gain[:], op0=ALU.mult, op1=ALU.mult),
                     reads=[("acc", tl), "ss3", "m_gain"], writes=["h32"])
                if plevel < 3:
                    return
                P.atom_begin()
                for c in range(8):
                    P.op("pe", lambda e, c=c: e.transpose(out=ptr[:, c, :], in_=h32[:, c * 128:(c + 1) * 128],
                                                          identity=K.ident32[:]),
                         reads=["h32"], writes=["ps_ptr"])
                P.op("act", lambda e: e.copy(out=hT32[:], in_=ptr[:]), reads=["ps_ptr"], writes=["hT32"])
                P.op("dve", lambda e, tl=tl: e.tensor_copy(out=hT[:, :, tl * 128:(tl + 1) * 128], in_=ptr[:]),
                     reads=["ps_ptr"], writes=[("hT", tl // 4)])
                P.atom_end()
                if plevel < 4:
                    return
                P.atom_begin()
                for c in range(8):
                    P.op("pe", lambda e, c=c: e.matmul(pY[1][:, 0:20], lhsT=hT32[:, c, :], rhs=wr[:, c, :],
                                                       start=(c == 0), stop=(c == 7)),
                         reads=["hT32", "m_wr"], writes=["ps_Y1"])
                P.op("dve", lambda e, tl=tl: e.tensor_tensor(out=Lall[:, tl, :], in0=pY[1][:, 0:20], in1=rb[:],
                                                             op=ALU.add),
                     reads=["ps_Y1", "m_rb"], writes=["Lall"])
                P.atom_end()


            def cap_prep(tl, ns):
                P.begin_capture(ns)
                prep_tile(tl)
                return P.end_capture()

            for tp in range(SBT // 2):
                P.replay([cap_prep(2 * tp, 0), cap_prep(2 * tp + 1, 1)])

            if stage >= 1:
                LG = Lall[:, :, 0:4]
                LE = Lall[:, :, 4:20]

                def b3(ap2, n):
                    return ap2.unsqueeze(2).to_broadcast([128, SBT, n])

                P.op("dve", lambda e: e.tensor_reduce(out=r_gmax[:], in_=LG, axis=AX.X, op=ALU.max),
                     reads=["Lall"], writes=["r_gmax"])
                P.op("dve", lambda e: e.tensor_tensor(out=r_gsh[:], in0=LG, in1=b3(r_gmax[:], 4), op=ALU.subtract),
                     reads=["Lall", "r_gmax"], writes=["r_gsh"])
                P.op("act", lambda e: e.activation(out=r_gexp[:], in_=r_gsh[:], func=AF.Exp),
                     reads=["r_gsh"], writes=["r_gexp"])
                P.op("dve", lambda e: e.tensor_reduce(out=r_gsum[:], in_=r_gexp[:], axis=AX.X, op=ALU.add),
                     reads=["r_gexp"], writes=["r_gsum"])
                P.op("dve", lambda e: e.reciprocal(out=r_gp[:], in_=r_gsum[:]), reads=["r_gsum"], writes=["r_gp"])
                P.op("dve", lambda e: e.tensor_scalar(out=r_pen[:], in0=r_gsh[:], scalar1=0.0, scalar2=None,
                                                      op0=ALU.is_ge), reads=["r_gsh"], writes=["r_pen"])
                P.op("dve", lambda e: e.tensor_scalar(out=r_pen[:], in0=r_pen[:], scalar1=-NEG, scalar2=NEG,
                                                      op0=ALU.mult, op1=ALU.add), reads=["r_pen"], writes=["r_pen"])
                P.op("dve", lambda e: e.tensor_tensor(
                    out=r_em[:].rearrange("p t (g j) -> p t g j", g=4),
                    in0=LE.rearrange("p t (g j) -> p t g j", g=4),
                    in1=r_pen[:].unsqueeze(3).to_broadcast([128, SBT, 4, 4]), op=ALU.add),
                     reads=["Lall", "r_pen"], writes=["r_em"])
                P.op("dve", lambda e: e.tensor_reduce(out=r_m1[:], in_=r_em[:], axis=AX.X, op=ALU.max),
                     reads=["r_em"], writes=["r_m1"])
                P.op("dve", lambda e: e.tensor_tensor(out=r_em[:], in0=r_em[:], in1=b3(r_m1[:], 16), op=ALU.subtract),
                     reads=["r_em", "r_m1"], writes=["r_em"])
                P.op("dve", lambda e: e.tensor_scalar(out=r_mask1[:], in0=r_em[:], scalar1=0.0, scalar2=None,
                                                      op0=ALU.is_ge), reads=["r_em"], writes=["r_mask1"])
                P.op("dve", lambda e: e.scalar_tensor_tensor(out=r_em2[:], in0=r_mask1[:], scalar=NEG, in1=r_em[:],
                                                             op0=ALU.mult, op1=ALU.add),
                     reads=["r_mask1", "r_em"], writes=["r_em2"])
                P.op("dve", lambda e: e.tensor_reduce(out=r_m2[:], in_=r_em2[:], axis=AX.X, op=ALU.max),
                     reads=["r_em2"], writes=["r_m2"])
                P.op("dve", lambda e: e.tensor_tensor(out=r_mask2[:], in0=r_em2[:], in1=b3(r_m2[:], 16), op=ALU.is_ge),
                     reads=["r_em2", "r_m2"], writes=["r_mask2"])
                P.op("act", lambda e: e.activation(out=r_ed[:], in_=r_m2[:], func=AF.Exp),
                     reads=["r_m2"], writes=["r_ed"])
                P.op("dve", lambda e: e.tensor_scalar(out=r_ed[:], in0=r_ed[:], scalar1=1.0, scalar2=None, op0=ALU.add),
                     reads=["r_ed"], writes=["r_ed"])
                P.op("dve", lambda e: e.reciprocal(out=r_ed[:], in_=r_ed[:]), reads=["r_ed"], writes=["r_ed"])
                P.op("dve", lambda e: e.tensor_tensor(out=r_w1[:], in0=r_gp[:], in1=r_ed[:], op=ALU.mult),
                     reads=["r_gp", "r_ed"], writes=["r_w1"])
                P.op("dve", lambda e: e.tensor_tensor(out=r_w2[:], in0=r_gp[:], in1=r_w1[:], op=ALU.subtract),
                     reads=["r_gp", "r_w1"], writes=["r_w2"])
                P.op("dve", lambda e: e.tensor_tensor(out=r_mask1[:], in0=r_mask1[:], in1=b3(r_w1[:], 16), op=ALU.mult),
                     reads=["r_mask1", "r_w1"], writes=["r_mask1"])
                P.op("dve", lambda e: e.tensor_tensor(out=r_mask2[:], in0=r_mask2[:], in1=b3(r_w2[:], 16), op=ALU.mult),
                     reads=["r_mask2", "r_w2"], writes=["r_mask2"])
                P.op("dve", lambda e: e.tensor_tensor(out=gates[:], in0=r_mask1[:], in1=r_mask2[:], op=ALU.add),
                     reads=["r_mask1", "r_mask2"], writes=["gates"])

            if sbi == 0:
                K.tap('gates', gates[:], [128, SBT, 16], ['gates'])
                K.tap('Lall', Lall[:], [128, SBT, 20], ['Lall'])
            if stage >= 2 and sbi == 0:
                load_w(0, 0)
            for ex in range(n_exp if stage >= 2 else 0):
                buf = ex % 2
                for b in range(SBT // 4):
                    emit_gu(ex, buf, b, nblk[0])
                    if pend[0] is not None:
                        emit_y(*pend[0])
                    pend[0] = (ex, buf, b, nblk[0])
                    nblk[0] += 1
                    if b == 0:
                        if ex + 1 < n_exp:
                            load_w(ex + 1, 1 - buf)
                        elif sbi + 1 < NSB:
                            load_w(0, 1 - buf)
            if pend[0] is not None:
                emit_y(*pend[0])
                pend[0] = None
            for tl in range(SBT):
                t = sbi * SBT + tl
                P.dma("sp", lambda e, t=t, tl=tl: e.dma_start(out=x_out[t * 128:(t + 1) * 128, :], in_=acc[:, tl, :]),
                      reads=[("acc", tl)], writes=[("xout", t)])
        P.barrier()
        P.flush(K.block, K.esem, K.dsem)


def bc(ap, pos, n):
    return ap.unsqueeze(pos).to_broadcast(list(ap.shape[:pos]) + [n] + list(ap.shape[pos:]))


class Phase:
    def __init__(self, K):
        self.K = K
        self.es = ExitStack()
        self.P = K.P
        K.uid = getattr(K, "uid", 0) + 1
        self.sfx = "_u%d" % K.uid

    def __enter__(self):
        self.es.__enter__()
        return self

    def __exit__(self, *a):
        self.P.barrier()
        self.P.flush(self.K.block, self.K.esem, self.K.dsem)
        return self.es.__exit__(*a)

    def sb(self, name, shape, dt=F32):
        return self.es.enter_context(self.K.nc.sbuf_tensor(name + self.sfx, shape, dt))

    def ps(self, name, shape, dt=F32):
        return self.es.enter_context(self.K.nc.psum_tensor(name + self.sfx, shape, dt))

    def rstd(self, eng2, ss, out, invd, n, key_in, key_out, tmp):
        P = self.P
        if isinstance(invd, float):
            P.op("dve", lambda e: e.tensor_scalar(out=tmp, in0=ss, scalar1=invd, scalar2=EPS, op0=ALU.mult,
                                                  op1=ALU.add), reads=[key_in], writes=[key_out + "_t"])
        else:
            P.op("dve", lambda e: e.tensor_tensor(out=tmp, in0=ss, in1=invd, op=ALU.mult),
                 reads=[key_in], writes=[key_out + "_t"])
            P.op("dve", lambda e: e.tensor_scalar(out=tmp, in0=tmp, scalar1=EPS, scalar2=None, op0=ALU.add),
                 reads=[key_out + "_t"], writes=[key_out + "_t"])
        P.op("act", lambda e: e.activation(out=tmp, in_=tmp, func=AF.Sqrt), reads=[key_out + "_t"],
             writes=[key_out + "_t"])
        P.op("dve", lambda e: e.reciprocal(out=out, in_=tmp), reads=[key_out + "_t"], writes=[key_out])

    def rope(self, x1, x2, cos, sin, o1, o2, t, rkeys, wkey, eng="dve"):
        P = self.P
        k = [wkey + "_t%d" % i for i in range(4)]
        P.op(eng, lambda e: e.tensor_tensor(out=t[0], in0=x1, in1=cos, op=ALU.mult), reads=rkeys, writes=[k[0]])
        P.op(eng, lambda e: e.tensor_tensor(out=t[1], in0=x2, in1=sin, op=ALU.mult), reads=rkeys, writes=[k[1]])
        P.op(eng, lambda e: e.tensor_tensor(out=t[2], in0=x2, in1=cos, op=ALU.mult), reads=rkeys, writes=[k[2]])
        P.op(eng, lambda e: e.tensor_tensor(out=t[3], in0=x1, in1=sin, op=ALU.mult), reads=rkeys, writes=[k[3]])
        P.op(eng, lambda e: e.tensor_tensor(out=o1, in0=t[0], in1=t[1], op=ALU.subtract),
             reads=[k[0], k[1]], writes=[wkey + "_o1"])
        P.op(eng, lambda e: e.tensor_tensor(out=o2, in0=t[2], in1=t[3], op=ALU.add),
             reads=[k[2], k[3]], writes=[wkey + "_o2"])
        return [wkey + "_o1", wkey + "_o2"]


def load_bc(ph, name, src_row, n):
    t = ph.sb(name, [128, n], F32)
    ph.P.dma("sp", lambda e: e.dma_start(out=t[:], in_=bc_rows(src_row)), writes=[name])
    return t


def l0_prep(K, x_in):
    nc, P = K.nc, K.P
    QT, KT, Vd = K.QT, K.KT, K.Vd
    with Phase(K) as ph:
        sb, ps = ph.sb, ph.ps
        Wab = sb("a_Wab", [128, 8, 1184], BF16)
        Wq = sb("a_Wq", [128, 2, 768], BF16)
        Wkv = sb("a_Wkv", [128, 1024], BF16)
        P.dma("pool", lambda e: e.dma_start(out=Wab[:], in_=K.w_in_ab[0].rearrange("(c p) n -> p c n", p=128)), writes=["Wab"])
        P.dma("pool", lambda e: e.dma_start(out=Wq[:], in_=K.mla_w_q_up[0].rearrange("(c p) n -> p c n", p=128)), writes=["Wq"])
        P.dma("pool", lambda e: e.dma_start(out=Wkv[:], in_=K.mla_w_kv_up[0]), writes=["Wkv"])
        gmix = load_bc(ph, "a_gmix", K.mix_norm[0:1, :], 1024)
        gqa = load_bc(ph, "a_gqa", K.mla_q_a_norm[0:1, :], 256)
        gkva = load_bc(ph, "a_gkva", K.mla_kv_a_norm[0:1, :], 128)
        gqn = load_bc(ph, "a_gqn", K.mla_qn_gain[0:1, :], 64)
        gkn = load_bc(ph, "a_gkn", K.mla_kn_gain[0:1, :], 64)
        gqr = load_bc(ph, "a_gqr", K.mla_qr_gain[0:1, :], 32)
        gkr = load_bc(ph, "a_gkr", K.mla_kr_gain[0:1, :], 32)
        gbq = load_bc(ph, "a_gbq", K.gqa_q_gain[0:1, :], 64)
        gbk = load_bc(ph, "a_gbk", K.gqa_k_gain[0:1, :], 64)
        gqk = sb("a_gqk", [128, 10, 64], F32)
        P.op("dve", lambda e: e.tensor_copy(out=gqk[:, 0:8, :], in_=bc(gbq[:], 1, 8)), reads=["a_gbq"], writes=["gqk"])
        P.op("dve", lambda e: e.tensor_copy(out=gqk[:, 8:10, :], in_=bc(gbk[:], 1, 2)), reads=["a_gbk"], writes=["gqk"])
        invA = sb("a_invA", [128, 13], F32)
        invB = sb("a_invB", [128, 24], F32)
        P.op("dve", lambda e: e.memset(invA[:, 0:1], 1.0 / 256), writes=["invA"])
        P.op("dve", lambda e: e.memset(invA[:, 1:2], 1.0 / 128), writes=["invA"])
        P.op("dve", lambda e: e.memset(invA[:, 2:3], 1.0 / 32), writes=["invA"])
        P.op("dve", lambda e: e.memset(invA[:, 3:13], 1.0 / 64), writes=["invA"])
        P.op("dve", lambda e: e.memset(invB[:, 0:8], 1.0 / 64), writes=["invB"])
        P.op("dve", lambda e: e.memset(invB[:, 8:16], 1.0 / 32), writes=["invB"])
        P.op("dve", lambda e: e.memset(invB[:, 16:24], 1.0 / 64), writes=["invB"])

        xt = sb("a_xt", [128, D], F32)
        junk = sb("a_junk", [128, D], BF16)
        ssx = sb("a_ssx", [128, 4], F32)
        xn = sb("a_xn", [128, D], BF16)
        xnT = sb("a_xnT", [128, 8, 128], BF16)
        prs = [sb("a_pr%d" % i, [128, 1184], F32) for i in range(4)]
        tabs = [sb("a_tab%d" % i, [128, 96], F32) for i in range(4)]
        QTs = [sb("a_QTs%d" % i, [128, 8, 512], BF16) for i in range(2)]
        KTs = [sb("a_KTs%d" % i, [128, 8, 512], BF16) for i in range(2)]
        BTs = [sb("a_BTs%d" % i, [128, 5, 512], BF16) for i in range(2)]

        SBB = []
        for _i in range(2):
            _d = {}
            _d["sqA"] = sb("a_sqA_%d" % _i, [128, 1056], F32)
            _d["stA"] = sb("a_stA_%d" % _i, [128, 13], F32)
            _d["tA"] = sb("a_tA_%d" % _i, [128, 13], F32)
            _d["rA"] = sb("a_rA_%d" % _i, [128, 13], F32)
            _d["kr"] = sb("a_kr_%d" % _i, [128, 32], F32)
            _d["krot"] = sb("a_krot_%d" % _i, [128, 32], F32)
            _d["bn"] = sb("a_bn_%d" % _i, [128, 10, 64], F32)
            _d["q32"] = sb("a_q32_%d" % _i, [128, 768], F32)
            _d["kv32"] = sb("a_kv32_%d" % _i, [128, 1024], F32)
            _d["sqB"] = sb("a_sqB_%d" % _i, [128, 768], F32)
            _d["sqK"] = sb("a_sqK_%d" % _i, [128, 8, 64], F32)
            _d["stB"] = sb("a_stB_%d" % _i, [128, 24], F32)
            _d["tB"] = sb("a_tB_%d" % _i, [128, 24], F32)
            _d["rB"] = sb("a_rB_%d" % _i, [128, 24], F32)
            _d["tmpn"] = sb("a_tmpn_%d" % _i, [128, 8, 64], F32)
            _d["qr"] = sb("a_qr_%d" % _i, [128, 8, 32], F32)
            _d["lat"] = sb("a_lat_%d" % _i, [128, 384], BF16)
            _d["latT"] = sb("a_latT_%d" % _i, [128, 3, 128], BF16)
            _d["Btok"] = sb("a_Btok_%d" % _i, [128, 10, 64], BF16)
            _d["Vb"] = sb("a_Vb_%d" % _i, [128, 2, 64], BF16)
            _d["Qtok"] = sb("a_Qtok_%d" % _i, [128, 8, 96], BF16)
            _d["Ktok"] = sb("a_Ktok_%d" % _i, [128, 8, 96], BF16)
            _d["Vm"] = sb("a_Vm_%d" % _i, [128, 8, 64], BF16)
            _d["rtk"] = [sb("a_rtk%d_%d" % (j, _i), [128, 16], F32) for j in range(4)]
            _d["rtb"] = [sb("a_rtb%d_%d" % (j, _i), [128, 320], F32) for j in range(4)]
            _d["rtq"] = [sb("a_rtq%d_%d" % (j, _i), [128, 128], F32) for j in range(4)]
            SBB.append(_d)
        ptb = ps("a_ptb", [128, 8, 128], BF16)
        pp = [ps("a_pp%d" % i, [128, 512], F32) for i in range(3)]
        pq = ps("a_pq", [128, 2, 512], F32)
        pkv = ps("a_pkv", [128, 2, 512], F32)
        ident = K.identb

        def stageA(t):
            pb = t % 4
            pr = prs[pb]
            tab = tabs[pb]
            P.dma("sp", lambda e, t=t: e.dma_start(out=xt[:], in_=x_in[t * 128:(t + 1) * 128, :]), writes=["xt"])
            P.dma("sp", lambda e, t=t: e.dma_start(out=tab[:], in_=K.ropetab[t * 128:(t + 1) * 128, :]), writes=[("tab", pb)])
            P.op("act", lambda e: e.activation(out=junk[:], in_=xt[:], func=AF.Square, accum_out=ssx[:, 0:1]),
                 reads=["xt"], writes=["junk", "ssx"])
            ph.rstd("act", ssx[:, 0:1], ssx[:, 2:3], 1.0 / D, 1, "ssx", "rx", ssx[:, 1:2])
            P.op("dve", lambda e: e.scalar_tensor_tensor(out=xn[:], in0=xt[:], scalar=ssx[:, 2:3], in1=gmix[:],
                                                         op0=ALU.mult, op1=ALU.mult),
                 reads=["xt", "rx", "a_gmix"], writes=["xn"])
            P.atom_begin()
            for c in range(8):
                P.op("pe", lambda e, c=c: e.transpose(out=ptb[:, c, :], in_=xn[:, c * 128:(c + 1) * 128], identity=ident[:]),
                     reads=["xn"], writes=["ps_tb"])
            P.op("act", lambda e: e.copy(out=xnT[:], in_=ptb[:]), reads=["ps_tb"], writes=["xnT"])
            P.atom_end()
            groups = [(0, 512), (512, 1024), (1024, 1184)]
            for g, (n0, n1) in enumerate(groups):
                for c in range(8):
                    P.op("pe", lambda e, c=c, g=g, n0=n0, n1=n1: e.matmul(pp[g][:, 0:n1 - n0], lhsT=xnT[:, c, :],
                                                                         rhs=Wab[:, c, n0:n1], start=(c == 0), stop=(c == 7)),
                         reads=["xnT", "Wab"], writes=[("ps_pp", g)])
            P.op("act", lambda e: e.copy(out=pr[:, 0:512], in_=pp[0][:]), reads=[("ps_pp", 0)], writes=[("pr", pb, 0)])
            P.op("dve", lambda e: e.tensor_copy(out=pr[:, 512:1024], in_=pp[1][:]), reads=[("ps_pp", 1)], writes=[("pr", pb, 1)])
            P.op("act", lambda e: e.copy(out=pr[:, 1024:1184], in_=pp[2][:, 0:160]), reads=[("ps_pp", 2)], writes=[("pr", pb, 2)])

        def stageB(t):
            pb = t % 4
            _L = SBB[t % 2]
            sqA = _L["sqA"]
            stA = _L["stA"]
            tA = _L["tA"]
            rA = _L["rA"]
            kr = _L["kr"]
            krot = _L["krot"]
            bn = _L["bn"]
            q32 = _L["q32"]
            kv32 = _L["kv32"]
            sqB = _L["sqB"]
            sqK = _L["sqK"]
            stB = _L["stB"]
            tB = _L["tB"]
            rB = _L["rB"]
            tmpn = _L["tmpn"]
            qr = _L["qr"]
            lat = _L["lat"]
            latT = _L["latT"]
            Btok = _L["Btok"]
            Vb = _L["Vb"]
            Qtok = _L["Qtok"]
            Ktok = _L["Ktok"]
            Vm = _L["Vm"]
            rtk = _L["rtk"]
            rtb = _L["rtb"]
            rtq = _L["rtq"]
            pr = prs[pb]
            tab = tabs[pb]
            stg = (t // 4) % 2
            tq = t % 4
            c0 = tq * 128
            PR = [("pr", pb, 0), ("pr", pb, 1), ("pr", pb, 2)]
            P.op("pool", lambda e: e.tensor_tensor(out=sqA[:], in0=pr[:, 0:1056], in1=pr[:, 0:1056], op=ALU.mult),
                 reads=PR, writes=["sqA"])
            P.op("dve", lambda e: e.tensor_reduce(out=stA[:, 0:1], in_=sqA[:, 0:256], axis=AX.X, op=ALU.add), reads=["sqA"], writes=["stA0"])
            P.op("dve", lambda e: e.tensor_reduce(out=stA[:, 1:2], in_=sqA[:, 256:384], axis=AX.X, op=ALU.add), reads=["sqA"], writes=["stA1"])
            P.op("dve", lambda e: e.tensor_reduce(out=stA[:, 2:3], in_=sqA[:, 384:416], axis=AX.X, op=ALU.add), reads=["sqA"], writes=["stA2"])
            P.op("dve", lambda e: e.tensor_reduce(out=stA[:, 3:13], in_=sqA[:, 416:1056].rearrange("p (h d) -> p h d", d=64),
                                                  axis=AX.X, op=ALU.add), reads=["sqA"], writes=["stA3"])
            P.op("dve", lambda e: e.tensor_tensor(out=tA[:], in0=stA[:], in1=invA[:], op=ALU.mult),
                 reads=["stA0", "stA1", "stA2", "stA3", "invA"], writes=["tA"])
            P.op("dve", lambda e: e.tensor_scalar(out=tA[:], in0=tA[:], scalar1=EPS, scalar2=None, op0=ALU.add), reads=["tA"], writes=["tA"])
            P.op("act", lambda e: e.activation(out=tA[:], in_=tA[:], func=AF.Sqrt), reads=["tA"], writes=["tA"])
            P.op("dve", lambda e: e.reciprocal(out=rA[:], in_=tA[:]), reads=["tA"], writes=["rA"])
            P.op("dve", lambda e: e.scalar_tensor_tensor(out=lat[:, 0:256], in0=pr[:, 0:256], scalar=rA[:, 0:1], in1=gqa[:],
                                                         op0=ALU.mult, op1=ALU.mult), reads=PR + ["rA", "a_gqa"], writes=["lat0"])
            P.op("dve", lambda e: e.scalar_tensor_tensor(out=lat[:, 256:384], in0=pr[:, 256:384], scalar=rA[:, 1:2], in1=gkva[:],
                                                         op0=ALU.mult, op1=ALU.mult), reads=PR + ["rA", "a_gkva"], writes=["lat1"])
            P.atom_begin()
            for c in range(3):
                P.op("pe", lambda e, c=c: e.transpose(out=ptb[:, c, :], in_=lat[:, c * 128:(c + 1) * 128], identity=ident[:]),
                     reads=["lat0", "lat1"], writes=["ps_tb"])
            P.op("act", lambda e: e.copy(out=latT[:], in_=ptb[:, 0:3, :]), reads=["ps_tb"], writes=["latT"])
            for g in range(2):
                for c in range(2):
                    P.op("pe", lambda e, c=c, g=g: e.matmul(pq[:, g, 0:384], lhsT=latT[:, c, :], rhs=Wq[:, c, g * 384:(g + 1) * 384],
                                                          start=(c == 0), stop=(c == 1)), reads=["latT", "Wq"], writes=[("ps_pq", g)])
            for g in range(2):
                P.op("pe", lambda e, g=g: e.matmul(pkv[:, g, :], lhsT=latT[:, 2, :], rhs=Wkv[:, g * 512:(g + 1) * 512],
                                                   start=True, stop=True), reads=["latT", "Wkv"], writes=[("ps_pkv", g)])
            P.op("act", lambda e: e.copy(out=q32[:, 0:384], in_=pq[:, 0, 0:384]), reads=[("ps_pq", 0)], writes=[("q32", 0)])
            P.op("dve", lambda e: e.tensor_copy(out=q32[:, 384:768], in_=pq[:, 1, 0:384]), reads=[("ps_pq", 1)], writes=[("q32", 1)])
            P.op("act", lambda e: e.copy(out=kv32[:, 0:512], in_=pkv[:, 0, :]), reads=[("ps_pkv", 0)], writes=[("kv32", 0)])
            P.op("dve", lambda e: e.tensor_copy(out=kv32[:, 512:1024], in_=pkv[:, 1, :]), reads=[("ps_pkv", 1)], writes=[("kv32", 1)])
            P.atom_end()
            Q32 = [("q32", 0), ("q32", 1)]
            KV32 = [("kv32", 0), ("kv32", 1)]
            q32v = q32[:].rearrange("p (h d) -> p h d", d=96)
            kv32v = kv32[:].rearrange("p (h d) -> p h d", d=128)
            P.op("dve", lambda e: e.scalar_tensor_tensor(out=kr[:], in0=pr[:, 384:416], scalar=rA[:, 2:3], in1=gkr[:],
                                                         op0=ALU.mult, op1=ALU.mult), reads=PR + ["rA", "a_gkr"], writes=["kr"])
            kk = ph.rope(kr[:, 0:16], kr[:, 16:32], tab[:, 0:16], tab[:, 16:32], krot[:, 0:16], krot[:, 16:32],
                         [rtk[i][:] for i in range(4)], ["kr", ("tab", pb)], "krot")
            P.op("pool", lambda e: e.tensor_tensor(out=bn[:], in0=pr[:, 416:1056].rearrange("p (h d) -> p h d", d=64),
                                                   in1=bc(rA[:, 3:13], 2, 64), op=ALU.mult), reads=PR + ["rA"], writes=["bn"])
            P.op("pool", lambda e: e.tensor_tensor(out=bn[:], in0=bn[:], in1=gqk[:], op=ALU.mult), reads=["bn", "gqk"], writes=["bn"])
            bnv = bn[:].rearrange("p h (a b f) -> p h a b f", a=2, b=2)
            Bv = Btok[:].rearrange("p h (a b f) -> p h a b f", a=2, b=2)
            tabv = tab[:, 32:96].rearrange("p (a b f) -> p a b f", a=2, b=2)
            cosb = bc(tabv[:, :, 0, :], 1, 10)
            sinb = bc(tabv[:, :, 1, :], 1, 10)
            rtv = [rtb[i][:].rearrange("p (h a f) -> p h a f", h=10, a=2) for i in range(4)]
            bkeys = ph.rope(bnv[:, :, :, 0, :], bnv[:, :, :, 1, :], cosb, sinb, Bv[:, :, :, 0, :], Bv[:, :, :, 1, :],
                            rtv, ["bn", ("tab", pb)], "Btok", eng="pool")
            P.op("act", lambda e: e.copy(out=Vb[:], in_=pr[:, 1056:1184].rearrange("p (h d) -> p h d", d=64)),
                 reads=PR, writes=["Vb"])
            P.op("pool", lambda e: e.tensor_tensor(out=sqB[:], in0=q32[:], in1=q32[:], op=ALU.mult), reads=Q32, writes=["sqB"])
            P.op("pool", lambda e: e.tensor_tensor(out=sqK[:], in0=kv32v[:, :, 0:64], in1=kv32v[:, :, 0:64], op=ALU.mult),
                 reads=KV32, writes=["sqK"])
            sqBv = sqB[:].rearrange("p (h d) -> p h d", d=96)
            P.op("dve", lambda e: e.tensor_reduce(out=stB[:, 0:8], in_=sqBv[:, :, 0:64], axis=AX.X, op=ALU.add), reads=["sqB"], writes=["stB0"])
            P.op("dve", lambda e: e.tensor_reduce(out=stB[:, 8:16], in_=sqBv[:, :, 64:96], axis=AX.X, op=ALU.add), reads=["sqB"], writes=["stB1"])
            P.op("dve", lambda e: e.tensor_reduce(out=stB[:, 16:24], in_=sqK[:], axis=AX.X, op=ALU.add), reads=["sqK"], writes=["stB2"])
            P.op("dve", lambda e: e.tensor_tensor(out=tB[:], in0=stB[:], in1=invB[:], op=ALU.mult),
                 reads=["stB0", "stB1", "stB2", "invB"], writes=["tB"])
            P.op("dve", lambda e: e.tensor_scalar(out=tB[:], in0=tB[:], scalar1=EPS, scalar2=None, op0=ALU.add), reads=["tB"], writes=["tB"])
            P.op("act", lambda e: e.activation(out=tB[:], in_=tB[:], func=AF.Sqrt), reads=["tB"], writes=["tB"])
            P.op("dve", lambda e: e.reciprocal(out=rB[:], in_=tB[:]), reads=["tB"], writes=["rB"])
            P.op("dve", lambda e: e.tensor_tensor(out=tmpn[:], in0=q32v[:, :, 0:64], in1=bc(rB[:, 0:8], 2, 64), op=ALU.mult),
                 reads=Q32 + ["rB"], writes=["tmpn"])
            P.op("dve", lambda e: e.tensor_tensor(out=Qtok[:, :, 0:64], in0=tmpn[:], in1=bc(gqn[:], 1, 8), op=ALU.mult),
                 reads=["tmpn", "a_gqn"], writes=["Qtok_n"])
            P.op("dve", lambda e: e.tensor_tensor(out=qr[:], in0=q32v[:, :, 64:96], in1=bc(rB[:, 8:16], 2, 32), op=ALU.mult),
                 reads=Q32 + ["rB"], writes=["qr"])
            P.op("dve", lambda e: e.tensor_tensor(out=qr[:], in0=qr[:], in1=bc(gqr[:], 1, 8), op=ALU.mult),
                 reads=["qr", "a_gqr"], writes=["qr"])
            qrv = qr[:].rearrange("p h (b f) -> p h b f", b=2)
            Qrv = Qtok[:, :, 64:96].rearrange("p h (b f) -> p h b f", b=2)
            rt8 = [rtq[i][:].rearrange("p (h f) -> p h f", h=8) for i in range(4)]
            qkeys = ph.rope(qrv[:, :, 0, :], qrv[:, :, 1, :], bc(tab[:, 0:16], 1, 8), bc(tab[:, 16:32], 1, 8),
                            Qrv[:, :, 0, :], Qrv[:, :, 1, :], rt8, ["qr", ("tab", pb)], "Qtok_r")
            P.op("dve", lambda e: e.tensor_tensor(out=tmpn[:], in0=kv32v[:, :, 0:64], in1=bc(rB[:, 16:24], 2, 64), op=ALU.mult),
                 reads=KV32 + ["rB", "Qtok_n"], writes=["tmpn"])
            P.op("dve", lambda e: e.tensor_tensor(out=Ktok[:, :, 0:64], in0=tmpn[:], in1=bc(gkn[:], 1, 8), op=ALU.mult),
                 reads=["tmpn", "a_gkn"], writes=["Ktok_n"])
            P.op("act", lambda e: e.copy(out=Ktok[:, :, 64:96], in_=bc(krot[:], 1, 8)), reads=kk, writes=["Ktok_r"])
            P.op("act", lambda e: e.copy(out=Vm[:], in_=kv32v[:, :, 64:128]), reads=KV32, writes=["Vm"])
            P.atom_begin()
            for h in range(8):
                P.op("pe", lambda e, h=h: e.transpose(out=ptb[0:96, h, :], in_=Qtok[:, h, :], identity=ident[:]),
                     reads=["Qtok_n"] + qkeys, writes=["ps_tb"])
            P.op("dve", lambda e, stg=stg, c0=c0: e.tensor_copy(out=QTs[stg][0:96, :, c0:c0 + 128], in_=ptb[0:96, :, :]),
                 reads=["ps_tb"], writes=[("QTs", stg)])
            for h in range(8):
                P.op("pe", lambda e, h=h: e.transpose(out=ptb[0:96, h, :], in_=Ktok[:, h, :], identity=ident[:]),
                     reads=["Ktok_n", "Ktok_r"], writes=["ps_tb"])
            P.op("act", lambda e, stg=stg, c0=c0: e.copy(out=KTs[stg][0:96, :, c0:c0 + 128], in_=ptb[0:96, :, :]),
                 reads=["ps_tb"], writes=[("KTs", stg)])
            Bflat = Btok[:].rearrange("p h d -> p (h d)")
            for j in range(5):
                P.op("pe", lambda e, j=j: e.transpose(out=ptb[:, j, :], in_=Bflat[:, j * 128:(j + 1) * 128], identity=ident[:]),
                     reads=bkeys, writes=["ps_tb"])
            P.op("dve", lambda e, stg=stg, c0=c0: e.tensor_copy(out=BTs[stg][:, :, c0:c0 + 128], in_=ptb[:, 0:5, :]),
                 reads=["ps_tb"], writes=[("BTs", stg)])
            P.atom_end()
            P.dma("sp", lambda e, t=t: e.dma_start(out=Vd[0:8, :, t, :].rearrange("h p d -> p h d"), in_=Vm[:]),
                  reads=["Vm"], writes=[("Vd", t)])
            P.dma("sp", lambda e, t=t: e.dma_start(out=Vd[8:10, :, t, :].rearrange("h p d -> p h d"), in_=Vb[:]),
                  reads=["Vb"], writes=[("Vd", t)])
            if tq == 3:
                n0 = (t // 4) * 512
                P.dma("sp", lambda e, stg=stg, n0=n0: e.dma_start(out=QT[0:8, :, n0:n0 + 512].rearrange("h d n -> d h n"),
                                                                  in_=QTs[stg][0:96, :, :]), reads=[("QTs", stg)], writes=["QT"])
                P.dma("sp", lambda e, stg=stg, n0=n0: e.dma_start(out=KT[0:8, :, n0:n0 + 512].rearrange("h d n -> d h n"),
                                                                  in_=KTs[stg][0:96, :, :]), reads=[("KTs", stg)], writes=["KT"])
                for u in range(2):
                    P.dma("sp", lambda e, stg=stg, n0=n0, u=u: e.dma_start(
                        out=QT[8 + u:16:2, 0:64, n0:n0 + 512].rearrange("h d n -> d h n"),
                        in_=BTs[stg][u * 64:(u + 1) * 64, 0:4, :]), reads=[("BTs", stg)], writes=["QT"])
                    P.dma("sp", lambda e, stg=stg, n0=n0, u=u: e.dma_start(
                        out=KT[8 + u, 0:64, n0:n0 + 512], in_=BTs[stg][u * 64:(u + 1) * 64, 4, :]),
                        reads=[("BTs", stg)], writes=["KT"])


        def cap(fn, t, ns):
            P.begin_capture(ns)
            fn(t)
            return P.end_capture()

        stageA(0)
        stageA(1)
        for p in range(NT // 2):
            streams = [cap(stageB, 2 * p, 0), cap(stageB, 2 * p + 1, 1)]
            if 2 * p + 2 < NT:
                streams.append(cap(stageA, 2 * p + 2, None) + cap(stageA, 2 * p + 3, None))
            P.replay(streams)

def attn_full(K, heads):
    nc, P = K.nc, K.P
    QT, KT, Vd, OT = K.QT, K.KT, K.Vd, K.OT
    QB = 1024
    with Phase(K) as ph:
        sb, ps = ph.sb, ph.ps
        kT = [sb("f_kT%d" % i, [96, S], BF16) for i in range(2)]
        qT = [sb("f_qT%d" % i, [96, S], BF16) for i in range(2)]
        vs = [sb("f_v%d" % i, [128, NT, 128], BF16) for i in range(2)]
        vst = [sb("f_vst%d" % i, [128, NT, 64], BF16) for i in range(2)]
        pT = [sb("f_pT%d" % i, [128, QB], BF16) for i in range(3)]
        osb = [sb("f_osb%d" % i, [128, 512], F32) for i in range(4)]
        rec = sb("f_rec", [64, 512], F32)
        oTs = [sb("f_oTs%d" % i, [64, 512], BF16) for i in range(2)]
        sc = [ps("f_sc%d" % i, [128, QB], F32) for i in range(2)]
        oacc = [ps("f_oacc%d" % i, [128, 512], F32) for i in range(2)]
        for i in range(2):
            P.op("dve", lambda e, i=i: e.memset(vs[i][:, :, 64:128], 1.0), writes=[("vone", i)])
        iters = []
        for hi, (hq, kv, d, scale, orow) in enumerate(heads):
            for qb in range(S // QB):
                for kc in range(NT):
                    iters.append((hi, hq, kv, d, scale, orow, qb, kc))

        def load_head(hi):
            hq, kv, d, scale, orow = heads[hi]
            b = hi % 2
            P.dma("sp", lambda e: e.dma_start(out=qT[b][0:d, :], in_=QT[hq, 0:d, :]), reads=["QT"], writes=[("qT", b)])
            P.dma("sp", lambda e: e.dma_start(out=kT[b][0:d, :], in_=KT[kv, 0:d, :]), reads=["KT"], writes=[("kT", b)])
            P.dma("sp", lambda e: e.dma_start(out=vst[b][:], in_=Vd[kv]), reads=["Vd"], writes=[("vst", b)])
            P.op("dve", lambda e: e.tensor_copy(out=vs[b][:, :, 0:64], in_=vst[b][:]), reads=[("vst", b)], writes=[("vs", b)])

        def emit_qk(n):
            hi, hq, kv, d, scale, orow, qb, kc = iters[n]
            b = hi % 2
            si = n % 2
            pi = n % 3
            for hf in range(2):
                q0 = qb * QB + hf * 512
                P.op("pe", lambda e, hf=hf, q0=q0: e.matmul(
                    sc[si][:, hf * 512:(hf + 1) * 512], lhsT=kT[b][0:d, kc * 128:(kc + 1) * 128],
                    rhs=qT[b][0:d, q0:q0 + 512], start=True, stop=True),
                    reads=[("kT", b), ("qT", b)], writes=[("ps_sc", si)])
            P.op("act", lambda e: e.activation(out=pT[pi][:], in_=sc[si][:], func=AF.Exp, scale=scale),
                 reads=[("ps_sc", si)], writes=[("pT", pi)])

        fin = [0]

        def emit_pv(n):
            hi, hq, kv, d, scale, orow, qb, kc = iters[n]
            b = hi % 2
            pi = n % 3
            for hf in range(2):
                P.op("pe", lambda e, hf=hf: e.matmul(
                    oacc[hf][:, :], lhsT=vs[b][:, kc, :], rhs=pT[pi][:, hf * 512:(hf + 1) * 512],
                    start=(kc == 0), stop=(kc == NT - 1)),
                    reads=[("vs", b), ("vone", b), ("pT", pi)], writes=[("ps_oacc", hf)])
            if kc != NT - 1:
                return
            obs = []
            for hf in range(2):
                ob = fin[0] % 4
                fin[0] += 1
                obs.append(ob)
                P.op("dve", lambda e, hf=hf, ob=ob: e.tensor_copy(out=osb[ob][:], in_=oacc[hf][:]),
                     reads=[("ps_oacc", hf)], writes=[("osb", ob)])
            for hf in range(2):
                q0 = qb * QB + hf * 512
                ob = obs[hf]
                o2 = ob % 2
                P.op("dve", lambda e, ob=ob: e.reciprocal(out=rec[:], in_=osb[ob][64:128, :]), reads=[("osb", ob)], writes=["rec"])
                P.op("dve", lambda e, ob=ob, o2=o2: e.tensor_tensor(out=oTs[o2][:], in0=osb[ob][0:64, :], in1=rec[:], op=ALU.mult),
                     reads=[("osb", ob), "rec"], writes=[("oTs", o2)])
                P.dma("sp", lambda e, o2=o2, q0=q0: e.dma_start(out=OT[orow:orow + 64, q0:q0 + 512], in_=oTs[o2][:]),
                      reads=[("oTs", o2)], writes=["OT"])

        N = len(iters)
        load_head(0)
        if len(heads) > 1:
            load_head(1)
        emit_qk(0)
        for n in range(N):
            if n + 1 < N:
                if iters[n + 1][0] != iters[n][0] and iters[n + 1][0] + 1 < len(heads):
                    pass
                emit_qk(n + 1)
            emit_pv(n)
            if n + 1 < N and iters[n + 1][0] != iters[n][0] and iters[n][0] + 2 < len(heads):
                load_head(iters[n][0] + 2)


def out_proj(K, w_dram, x_in, x_out):
    nc, P = K.nc, K.P
    OT = K.OT
    with Phase(K) as ph:
        sb, ps = ph.sb, ph.ps
        Wo = sb("o_W", [128, 8, 1024], BF16)
        P.dma("pool", lambda e: e.dma_start(out=Wo[:], in_=w_dram.rearrange("(c p) n -> p c n", p=128)), writes=["Wo"])
        oT = [sb("o_oT%d" % i, [128, 8, 512], BF16) for i in range(2)]
        xt = [sb("o_xt%d" % i, [128, D], F32) for i in range(4)]
        po = [ps("o_po%d" % i, [128, 512], F32) for i in range(4)]
        for blk in range(S // 512):
            ob = blk % 2
            P.dma("sp", lambda e, ob=ob, blk=blk: e.dma_start(
                out=oT[ob][:], in_=OT[:, blk * 512:(blk + 1) * 512].rearrange("(c p) n -> p c n", p=128)),
                reads=["OT"], writes=[("oT", ob)])
            for s4 in range(4):
                t = blk * 4 + s4
                xb = t % 4
                P.dma("sp", lambda e, xb=xb, t=t: e.dma_start(out=xt[xb][:], in_=x_in[t * 128:(t + 1) * 128, :]), writes=[("xt", xb)])
                for nh in range(2):
                    pb = (t * 2 + nh) % 4
                    for c in range(8):
                        P.op("pe", lambda e, c=c, ob=ob, s4=s4, nh=nh, pb=pb: e.matmul(
                            po[pb][:], lhsT=oT[ob][:, c, s4 * 128:(s4 + 1) * 128], rhs=Wo[:, c, nh * 512:(nh + 1) * 512],
                            start=(c == 0), stop=(c == 7)), reads=[("oT", ob), "Wo"], writes=[("ps_po", pb)])
                    P.op("dve", lambda e, xb=xb, nh=nh, pb=pb: e.tensor_tensor(
                        out=xt[xb][:, nh * 512:(nh + 1) * 512], in0=po[pb][:], in1=xt[xb][:, nh * 512:(nh + 1) * 512], op=ALU.add),
                        reads=[("ps_po", pb), ("xt", xb)], writes=[("xt", xb)])
                P.dma("act", lambda e, xb=xb, t=t: e.dma_start(out=x_out[t * 128:(t + 1) * 128, :], in_=xt[xb][:]),
                      reads=[("xt", xb)], writes=[("xout", t)])


def l1_prep(K, x_in):
    nc, P = K.nc, K.P
    QT, KT, Vd = K.QT, K.KT, K.Vd
    with Phase(K) as ph:
        sb, ps = ph.sb, ph.ps
        Wc = sb("c_Wc", [128, 8, 1536], BF16)
        P.dma("pool", lambda e: e.dma_start(out=Wc[:], in_=K.w_in_c[0].rearrange("(c p) n -> p c n", p=128)), writes=["Wc"])
        gmix = load_bc(ph, "c_gmix", K.mix_norm[1:2, :], 1024)
        gq = load_bc(ph, "c_gq", K.win_q_gain[0:1, :], 64)
        gk = load_bc(ph, "c_gk", K.win_k_gain[0:1, :], 64)
        gqk = sb("c_gqk", [128, 20, 64], F32)
        P.op("dve", lambda e: e.tensor_copy(out=gqk[:, 0:16, :], in_=bc(gq[:], 1, 16)), reads=["c_gq"], writes=["gqk"])
        P.op("dve", lambda e: e.tensor_copy(out=gqk[:, 16:20, :], in_=bc(gk[:], 1, 4)), reads=["c_gk"], writes=["gqk"])
        xt = sb("c_xt", [128, D], F32)
        junk = sb("c_junk", [128, D], BF16)
        ssx = sb("c_ssx", [128, 4], F32)
        xn = sb("c_xn", [128, D], BF16)
        xnT = sb("c_xnT", [128, 8, 128], BF16)
        prs = [sb("c_pr%d" % i, [128, 1536], F32) for i in range(4)]
        BTs = [sb("c_BTs%d" % i, [128, 10, 512], BF16) for i in range(2)]
        SBB = []
        for _i in range(2):
            _d = {}
            _d["sq"] = sb("c_sq_%d" % _i, [128, 1280], F32)
            _d["st"] = sb("c_st_%d" % _i, [128, 20], F32)
            _d["tt"] = sb("c_tt_%d" % _i, [128, 20], F32)
            _d["rr"] = sb("c_rr_%d" % _i, [128, 20], F32)
            _d["bn"] = sb("c_bn_%d" % _i, [128, 20, 64], F32)
            _d["Btok"] = sb("c_Btok_%d" % _i, [128, 20, 64], BF16)
            _d["Vc"] = sb("c_Vc_%d" % _i, [128, 4, 64], BF16)
            SBB.append(_d)
        ptb = ps("c_ptb", [128, 8, 128], BF16)
        ptb2 = ps("c_ptb2", [128, 8, 128], BF16)
        pp = [ps("c_pp%d" % i, [128, 512], F32) for i in range(3)]
        ident = K.identb
        def stageA(t):
            pb = t % 4
            pr = prs[pb]
            P.dma("sp", lambda e, t=t: e.dma_start(out=xt[:], in_=x_in[t * 128:(t + 1) * 128, :]), writes=["xt"])
            P.op("act", lambda e: e.activation(out=junk[:], in_=xt[:], func=AF.Square, accum_out=ssx[:, 0:1]),
                 reads=["xt"], writes=["junk", "ssx"])
            ph.rstd("act", ssx[:, 0:1], ssx[:, 2:3], 1.0 / D, 1, "ssx", "rx", ssx[:, 1:2])
            P.op("dve", lambda e: e.scalar_tensor_tensor(out=xn[:], in0=xt[:], scalar=ssx[:, 2:3], in1=gmix[:],
                                                         op0=ALU.mult, op1=ALU.mult), reads=["xt", "rx", "c_gmix"], writes=["xn"])
            P.atom_begin()
            for c in range(8):
                P.op("pe", lambda e, c=c: e.transpose(out=ptb[:, c, :], in_=xn[:, c * 128:(c + 1) * 128], identity=ident[:]),
                     reads=["xn"], writes=["ps_tb"])
            P.op("act", lambda e: e.copy(out=xnT[:], in_=ptb[:]), reads=["ps_tb"], writes=["xnT"])
            P.atom_end()
            for g in range(3):
                for c in range(8):
                    P.op("pe", lambda e, c=c, g=g: e.matmul(pp[g][:], lhsT=xnT[:, c, :], rhs=Wc[:, c, g * 512:(g + 1) * 512],
                                                          start=(c == 0), stop=(c == 7)), reads=["xnT", "Wc"], writes=[("ps_pp", g)])
            P.op("act", lambda e: e.copy(out=pr[:, 0:512], in_=pp[0][:]), reads=[("ps_pp", 0)], writes=[("pr", pb, 0)])
            P.op("dve", lambda e: e.tensor_copy(out=pr[:, 512:1024], in_=pp[1][:]), reads=[("ps_pp", 1)], writes=[("pr", pb, 1)])
            P.op("act", lambda e: e.copy(out=pr[:, 1024:1536], in_=pp[2][:]), reads=[("ps_pp", 2)], writes=[("pr", pb, 2)])

        def stageB(t):
            pb = t % 4
            _L = SBB[t % 2]
            sq = _L["sq"]
            st = _L["st"]
            tt = _L["tt"]
            rr = _L["rr"]
            bn = _L["bn"]
            Btok = _L["Btok"]
            Vc = _L["Vc"]
            pr = prs[pb]
            stg = (t // 4) % 2
            c0 = (t % 4) * 128
            PR = [("pr", pb, 0), ("pr", pb, 1), ("pr", pb, 2)]
            prv = pr[:, 0:1280].rearrange("p (h d) -> p h d", d=64)
            P.op("pool", lambda e: e.tensor_tensor(out=sq[:], in0=pr[:, 0:1280], in1=pr[:, 0:1280], op=ALU.mult), reads=PR, writes=["sq"])
            P.op("dve", lambda e: e.tensor_reduce(out=st[:], in_=sq[:].rearrange("p (h d) -> p h d", d=64), axis=AX.X, op=ALU.add),
                 reads=["sq"], writes=["st"])
            ph.rstd("act", st[:], rr[:], 1.0 / 64, 20, "st", "rr", tt[:])
            P.op("dve", lambda e: e.tensor_tensor(out=bn[:], in0=prv, in1=bc(rr[:], 2, 64), op=ALU.mult), reads=PR + ["rr"], writes=["bn"])
            P.op("pool", lambda e: e.tensor_tensor(out=Btok[:], in0=bn[:], in1=gqk[:], op=ALU.mult), reads=["bn", "gqk"], writes=["Btok"])
            P.op("act", lambda e: e.copy(out=Vc[:], in_=pr[:, 1280:1536].rearrange("p (h d) -> p h d", d=64)), reads=PR, writes=["Vc"])
            Bflat = Btok[:].rearrange("p h d -> p (h d)")
            P.atom_begin()
            for j in range(8):
                P.op("pe", lambda e, j=j: e.transpose(out=ptb[:, j, :], in_=Bflat[:, j * 128:(j + 1) * 128], identity=ident[:]),
                     reads=["Btok"], writes=["ps_tb"])
            for j in range(2):
                P.op("pe", lambda e, j=j: e.transpose(out=ptb2[:, j, :], in_=Bflat[:, (8 + j) * 128:(9 + j) * 128], identity=ident[:]),
                     reads=["Btok"], writes=["ps_tb2"])
            P.op("dve", lambda e, stg=stg, c0=c0: e.tensor_copy(out=BTs[stg][:, 0:8, c0:c0 + 128], in_=ptb[:]),
                 reads=["ps_tb"], writes=[("BTs", stg)])
            P.op("act", lambda e, stg=stg, c0=c0: e.copy(out=BTs[stg][:, 8:10, c0:c0 + 128], in_=ptb2[:, 0:2, :]),
                 reads=["ps_tb2"], writes=[("BTs", stg)])
            P.atom_end()
            P.dma("sp", lambda e, t=t: e.dma_start(out=Vd[0:4, :, t, :].rearrange("h p d -> p h d"), in_=Vc[:]),
                  reads=["Vc"], writes=[("Vd", t)])
            if t % 4 == 3:
                n0 = (t // 4) * 512
                for u in range(2):
                    P.dma("sp", lambda e, stg=stg, n0=n0, u=u: e.dma_start(
                        out=QT[u:16:2, 0:64, n0:n0 + 512].rearrange("h d n -> d h n"),
                        in_=BTs[stg][u * 64:(u + 1) * 64, 0:8, :]), reads=[("BTs", stg)], writes=["QT"])
                    P.dma("sp", lambda e, stg=stg, n0=n0, u=u: e.dma_start(
                        out=KT[u:4:2, 0:64, n0:n0 + 512].rearrange("h d n -> d h n"),
                        in_=BTs[stg][u * 64:(u + 1) * 64, 8:10, :]), reads=[("BTs", stg)], writes=["KT"])


        def cap(fn, t, ns):
            P.begin_capture(ns)
            fn(t)
            return P.end_capture()

        stageA(0)
        stageA(1)
        for p in range(NT // 2):
            streams = [cap(stageB, 2 * p, 0), cap(stageB, 2 * p + 1, 1)]
            if 2 * p + 2 < NT:
                streams.append(cap(stageA, 2 * p + 2, None) + cap(stageA, 2 * p + 3, None))
            P.replay(streams)

def attn_win(K):
    nc, P = K.nc, K.P
    QT, KT, Vd, OT = K.QT, K.KT, K.Vd, K.OT
    scale = 64 ** -0.5
    with Phase(K) as ph:
        sb, ps = ph.sb, ph.ps
        kT = [sb("w_kT%d" % i, [64, S], BF16) for i in range(2)]
        qT = [sb("w_qT%d" % i, [64, S], BF16) for i in range(2)]
        vs = [sb("w_v%d" % i, [128, NT, 128], BF16) for i in range(2)]
        vst = [sb("w_vst%d" % i, [128, NT, 64], BF16) for i in range(2)]
        bm = [sb("w_bm%d" % i, [128, 3, 128], F32) for i in range(2)]
        mask = sb("w_mask", [128, 3, 128], F32)
        ssb = [sb("w_ssb%d" % i, [128, 4, 3, 128], F32) for i in range(2)]
        pT = [sb("w_pT%d" % i, [128, 4, 3, 128], BF16) for i in range(2)]
        esink = sb("w_esink", [128, 16], F32)
        osb = [sb("w_osb%d" % i, [128, 512], F32) for i in range(2)]
        lnd = sb("w_lnd", [64, 512], F32)
        rec = sb("w_rec", [64, 512], F32)
        oTs = [sb("w_oTs%d" % i, [64, 512], BF16) for i in range(2)]
        scw = [ps("w_sc%d" % i, [128, 4, 3, 128], F32) for i in range(2)]
        oacc = [ps("w_oacc%d" % i, [128, 512], F32) for i in range(2)]
        for i in range(2):
            P.op("dve", lambda e, i=i: e.memset(vs[i][:, :, 64:128], 1.0), writes=[("vone", i)])
            P.op("dve", lambda e, i=i: e.memset(ssb[i][:], 0.0), writes=[("ssb", i)])
        P.dma("sp", lambda e: e.dma_start(out=mask[:], in_=K.winmask[:, :, :]), writes=["mask"])
        P.dma("sp", lambda e: e.dma_start(out=esink[:], in_=bc_rows(K.win_sink[0:1, :])), writes=["esink"])
        P.op("act", lambda e: e.activation(out=esink[:], in_=esink[:], func=AF.Exp), reads=["esink"], writes=["esink"])

        def load_head(h):
            b = h % 2
            kv = h // 4
            P.dma("sp", lambda e: e.dma_start(out=qT[b][:, :], in_=QT[h, 0:64, :]), reads=["QT"], writes=[("qT", b)])
            P.dma("sp", lambda e: e.dma_start(out=kT[b][:, :], in_=KT[kv, 0:64, :]), reads=["KT"], writes=[("kT", b)])
            P.dma("sp", lambda e: e.dma_start(out=vst[b][:], in_=Vd[kv]), reads=["Vd"], writes=[("vst", b)])
            P.op("dve", lambda e: e.tensor_copy(out=vs[b][:, :, 0:64], in_=vst[b][:]), reads=[("vst", b)], writes=[("vs", b)])
            P.dma("sp", lambda e: e.dma_start(out=bm[b][:], in_=K.winbias[h]), writes=[("bm", b)])
            P.op("dve", lambda e: e.tensor_tensor(out=bm[b][:], in0=bm[b][:], in1=mask[:], op=ALU.add),
                 reads=[("bm", b), "mask"], writes=[("bm", b)])

        groups = [(h, g) for h in range(16) for g in range(8)]

        def valid_of(g):
            valid = {}
            for blk in range(4):
                i = g * 4 + blk
                for j in range(3):
                    kc = i - 1 + j
                    if 0 <= kc < NT:
                        valid[(blk, j)] = kc
            return valid

        def emit_qk(n):
            h, g = groups[n]
            b = h % 2
            si = n % 2
            valid = valid_of(g)
            for (blk, j), kc in valid.items():
                i = g * 4 + blk
                P.op("pe", lambda e, blk=blk, j=j, kc=kc, i=i: e.matmul(
                    scw[si][:, blk, j, :], lhsT=kT[b][:, kc * 128:(kc + 1) * 128], rhs=qT[b][:, i * 128:(i + 1) * 128],
                    start=True, stop=True), reads=[("kT", b), ("qT", b)], writes=[("ps_scw", si)])
            if len(valid) == 12:
                P.op("dve", lambda e: e.scalar_tensor_tensor(
                    out=ssb[si][:], in0=scw[si][:], scalar=scale, in1=bc(bm[b][:], 1, 4), op0=ALU.mult, op1=ALU.add),
                    reads=[("ps_scw", si), ("bm", b)], writes=[("ssb", si)])
            else:
                for blk in range(4):
                    js = [j for j in range(3) if (blk, j) in valid]
                    j0, j1 = js[0], js[-1] + 1
                    P.op("dve", lambda e, blk=blk, j0=j0, j1=j1: e.scalar_tensor_tensor(
                        out=ssb[si][:, blk, j0:j1, :], in0=scw[si][:, blk, j0:j1, :], scalar=scale,
                        in1=bm[b][:, j0:j1, :], op0=ALU.mult, op1=ALU.add),
                        reads=[("ps_scw", si), ("bm", b)], writes=[("ssb", si)])
            P.op("act", lambda e: e.activation(out=pT[si][:], in_=ssb[si][:], func=AF.Exp),
                 reads=[("ssb", si)], writes=[("pT", si)])

        def emit_pv(n):
            h, g = groups[n]
            b = h % 2
            si = n % 2
            ob = n % 2
            valid = valid_of(g)
            for blk in range(4):
                js = [j for j in range(3) if (blk, j) in valid]
                for j in js:
                    kc = valid[(blk, j)]
                    P.op("pe", lambda e, blk=blk, j=j, kc=kc, js=js: e.matmul(
                        oacc[ob][:, blk * 128:(blk + 1) * 128], lhsT=vs[b][:, kc, :], rhs=pT[si][:, blk, j, :],
                        start=(j == js[0]), stop=(j == js[-1])),
                        reads=[("vs", b), ("vone", b), ("pT", si)], writes=[("ps_oacc", ob)])
            P.op("dve", lambda e: e.tensor_copy(out=osb[ob][:], in_=oacc[ob][:]), reads=[("ps_oacc", ob)], writes=[("osb", ob)])
            P.op("act", lambda e: e.activation(out=lnd[:], in_=osb[ob][64:128, :], func=AF.Ln, bias=esink[64:128, h:h + 1]),
                 reads=[("osb", ob), "esink"], writes=["lnd"])
            P.op("act", lambda e: e.activation(out=rec[:], in_=lnd[:], func=AF.Exp, scale=-1.0), reads=["lnd"], writes=["rec"])
            P.op("dve", lambda e: e.tensor_tensor(out=oTs[ob][:], in0=osb[ob][0:64, :], in1=rec[:], op=ALU.mult),
                 reads=[("osb", ob), "rec"], writes=[("oTs", ob)])
            P.dma("sp", lambda e: e.dma_start(out=OT[h * 64:(h + 1) * 64, g * 512:(g + 1) * 512], in_=oTs[ob][:]),
                  reads=[("oTs", ob)], writes=["OT"])

        N = len(groups)
        load_head(0)
        load_head(1)
        emit_qk(0)
        for n in range(N):
            if n + 1 < N:
                emit_qk(n + 1)
            emit_pv(n)
            if n + 1 < N and groups[n + 1][0] != groups[n][0] and groups[n][0] + 2 < 16:
                load_head(groups[n][0] + 2)


W_NAMES = ["mix_norm", "ffn_norm", "w_in_ab", "mla_q_a_norm", "mla_w_q_up", "mla_kv_a_norm",
           "mla_w_kv_up", "mla_qn_gain", "mla_kn_gain", "mla_qr_gain", "mla_kr_gain",
           "gqa_q_gain", "gqa_k_gain", "w_out_ab", "w_in_c", "win_q_gain", "win_k_gain",
           "win_sink", "w_out_c", "rel_bias", "moe_w_group", "moe_b_group", "moe_w_router",
           "moe_b_router", "moe_w_gate", "moe_w_up", "moe_w_down"]

W_SHAPES = {
    "mix_norm": [2, 1024], "ffn_norm": [2, 1024], "w_in_ab": [1, 1024, 1184], "mla_q_a_norm": [1, 256],
    "mla_w_q_up": [1, 256, 768], "mla_kv_a_norm": [1, 128], "mla_w_kv_up": [1, 128, 1024],
    "mla_qn_gain": [1, 64], "mla_kn_gain": [1, 64], "mla_qr_gain": [1, 32], "mla_kr_gain": [1, 32],
    "gqa_q_gain": [1, 64], "gqa_k_gain": [1, 64], "w_out_ab": [1, 1024, 1024], "w_in_c": [1, 1024, 1536],
    "win_q_gain": [1, 64], "win_k_gain": [1, 64], "win_sink": [1, 16], "w_out_c": [1, 1024, 1024],
    "rel_bias": [32, 16], "moe_w_group": [2, 1024, 4], "moe_b_group": [2, 4], "moe_w_router": [2, 1024, 16],
    "moe_b_router": [2, 16], "moe_w_gate": [2, 16, 1024, 512], "moe_w_up": [2, 16, 1024, 512],
    "moe_w_down": [2, 16, 512, 1024],
}


def build_program(phases=("all",), dbg=None):
    nc = bass.Bass("TRN2", target_bir_lowering=False)
    K = Ctx()
    K.nc = nc
    K.dbg = dict(dbg or {})
    K.taps = K.dbg.pop('taps', ())
    K.P = Prog()
    K.x = nc.dram_tensor("x", [S, D], F32, kind="ExternalInput").ap()
    K.used_inputs = []
    K.ident_d = nc.dram_tensor("ident", [128, 128], F32, kind="ExternalInput").ap()
    K.out = nc.dram_tensor("out", [S, D], F32, kind="ExternalOutput").ap()
    K.ropetab = nc.dram_tensor("ropetab", [S, 96], F32, kind="ExternalInput").ap()
    K.winbias = nc.dram_tensor("winbias", [16, 128, 3, 128], F32, kind="ExternalInput").ap()
    K.winmask = nc.dram_tensor("winmask", [128, 3, 128], F32, kind="ExternalInput").ap()
    skind = "ExternalOutput" if K.dbg.get("scratch_out") else "Internal"
    K.QT = nc.dram_tensor("QT", [16, 96, S], BF16, kind=skind).ap()
    K.KT = nc.dram_tensor("KT", [10, 96, S], BF16, kind=skind).ap()
    K.Vd = nc.dram_tensor("Vd", [10, 128, NT, 64], BF16, kind=skind).ap()
    K.OT = nc.dram_tensor("OT", [1024, S], BF16, kind=skind).ap()
    with ExitStack() as es:
        K.ident32 = es.enter_context(nc.sbuf_tensor("ident32", [128, 128], F32))
        K.identb = es.enter_context(nc.sbuf_tensor("identb", [128, 128], BF16))
        K.esem = {e: es.enter_context(nc.semaphore("s_" + e)) for e in ENGS}
        K.dsem = [es.enter_context(nc.semaphore("d%d" % i)) for i in range(K.P.n_dma)]
        K.block = es.enter_context(nc.Block())
        P = K.P
        P.dma("sp", lambda e: e.dma_start(out=K.ident32[:], in_=K.ident_d[:, :]), writes=["ident32"])
        P.op("dve", lambda e: e.tensor_copy(out=K.identb[:], in_=K.ident32[:]), reads=["ident32"], writes=["identb"])
        P.barrier()
        P.flush(K.block, K.esem, K.dsem)
        if "all" in phases:
            l0_prep(K, K.x)
            heads = [(h, h, 96, 96 ** -0.5, h * 64) for h in range(8)] + \
                    [(8 + j, 8 + j // 4, 64, 64 ** -0.5, 512 + j * 64) for j in range(8)]
            attn_full(K, heads)
            out_proj(K, K.w_out_ab[0], K.x, K.out)
            moe_phase(K, 0, K.out, K.out)
            l1_prep(K, K.out)
            attn_win(K)
            out_proj(K, K.w_out_c[0], K.out, K.out)
            moe_phase(K, 1, K.out, K.out)
        if "moe0" in phases:
            moe_phase(K, 0, K.x, K.out, **K.dbg)
        if "mix0" in phases:
            l0_prep(K, K.x)
            heads = [(h, h, 96, 96 ** -0.5, h * 64) for h in range(8)] + \
                    [(8 + j, 8 + j // 4, 64, 64 ** -0.5, 512 + j * 64) for j in range(8)]
            heads = heads[:K.dbg.get("nheads", 16)]
            if K.dbg.get("attn", True):
                attn_full(K, heads)
                out_proj(K, K.w_out_ab[0], K.x, K.out)
        if "mix1" in phases:
            xin = K.x if "mix0" not in phases else K.out
            l1_prep(K, xin)
            attn_win(K)
            out_proj(K, K.w_out_c[0], xin, K.out)
    nc.used_inputs = list(K.used_inputs)
    return nc


def rope_table():
    inv = (np.float32(10000.0) ** (-np.arange(0, 32, 2, dtype=np.float32) / np.float32(32))).astype(np.float32)
    pos = np.arange(S)
    tabs = []
    for p in (pos, pos // 64, pos % 64):
        ang = p.astype(np.float32)[:, None] * inv[None, :]
        tabs += [np.cos(ang), np.sin(ang)]
    return np.ascontiguousarray(np.concatenate(tabs, axis=1).astype(np.float32))


def win_tables(rel_bias):
    kk = np.arange(128)[:, None, None]
    j = np.arange(3)[None, :, None]
    qi = np.arange(128)[None, None, :]
    rel = (j * 128 + kk) - 128 - qi
    nb, max_exact = 16, 8
    ret = np.where(rel > 0, nb, 0)
    n = np.abs(rel)
    nf = np.maximum(n, 1).astype(np.float32)
    large = max_exact + (np.log(nf / np.float32(max_exact)) / np.float32(math.log(128 / max_exact))
                         * np.float32(nb - max_exact)).astype(np.int32)
    large = np.minimum(large, nb - 1)
    bucket = ret + np.where(n < max_exact, n, large)
    bias = np.ascontiguousarray(np.transpose(rel_bias[bucket], (3, 0, 1, 2))).astype(np.float32)
    mask = np.where(np.abs(rel) <= 128, 0.0, -30000.0).astype(np.float32)
    return bias, np.ascontiguousarray(mask)


_NC_CACHE = {}


def kernel(**inputs):
    n_cores = 8
    if "nc" not in _NC_CACHE:
        _NC_CACHE["nc"] = build_program(phases=("all",))
    nc = _NC_CACHE["nc"]
    x = np.ascontiguousarray(np.asarray(inputs["x"], dtype=np.float32))
    shared = {"ident": np.eye(128, dtype=np.float32), "ropetab": rope_table()}
    wb, wm = win_tables(np.asarray(inputs["rel_bias"], dtype=np.float32))
    shared["winbias"] = wb
    shared["winmask"] = wm
    for n in nc.used_inputs:
        shared[n] = np.ascontiguousarray(np.asarray(inputs[n], dtype=np.float32))
    in_maps = []
    for i in range(n_cores):
        m = dict(shared)
        m["x"] = x[i]
        in_maps.append(m)
    res = run_bass_kernel_spmd(nc, in_maps, core_ids=list(range(n_cores)))
    return np.stack([np.asarray(r["out"], dtype=np.float32) for r in res.results], axis=0)
```

```python
from contextlib import ExitStack
import math
import numpy as np
import concourse.bass as bass
import concourse.mybir as mybir
from concourse.bass_utils import run_bass_kernel_spmd

F32 = mybir.dt.float32
BF16 = mybir.dt.bfloat16
ALU = mybir.AluOpType
AF = mybir.ActivationFunctionType
AX = mybir.AxisListType

ENGS = ("pe", "act", "dve", "pool", "sp")
S = 4096
D = 1024
NT = S // 128
EPS = 1e-6
NEG = -1.0e30
STRICT_SAME_ENGINE = True


class _Op:
    __slots__ = ("fn", "waits", "signal", "dma")

    def __init__(self, fn, waits, dma):
        self.fn = fn
        self.waits = waits
        self.signal = False
        self.dma = dma


class Prog:
    def __init__(self, n_dma_sems=32):
        self.ops = {e: [] for e in ENGS}
        self.done = {e: 0 for e in ENGS}
        self.sigbase = {e: 0 for e in ENGS}
        self.sigmap = {e: {} for e in ENGS}
        self.seen = {e: {} for e in ENGS}
        self.res = {}
        self.n_dma = n_dma_sems
        self.dma_cnt = [0] * n_dma_sems
        self.dma_rr = 0
        self.n_sw = 8
        self.sw_rr = 0

    def _deps(self, eng, reads, writes):
        deps = {}

        def add(p, kind):
            if p is None:
                return
            prod, tick = p
            if prod == eng and kind != "raw" and not STRICT_SAME_ENGINE:
                return
            if prod == "pe" and eng == "pe":
                return
            if tick > deps.get(prod, -1):
                deps[prod] = tick

        for k in reads:
            r = self.res.get(k)
            if r is not None:
                add(r[0], "raw")
        for k in writes:
            r = self.res.get(k)
            if r is not None:
                add(r[0], "waw")
                for rd in r[1]:
                    add(rd, "war")
        waits = []
        seen = self.seen[eng]
        for prod, tick in deps.items():
            if seen.get(prod, -1) >= tick:
                continue
            seen[prod] = tick
            waits.append((prod, tick))
            if not isinstance(prod, tuple):
                assert tick >= self.done[prod], "dependency on flushed op without barrier"
                self.ops[prod][tick - self.done[prod]].signal = True
        return waits

    def _record(self, me, reads, writes):
        for k in reads:
            r = self.res.get(k)
            if r is None:
                self.res[k] = [None, [me]]
            else:
                r[1].append(me)
        for k in writes:
            self.res[k] = [me, []]

    @staticmethod
    def _excl(reads, writes):
        ps = [k for k in reads if (k if isinstance(k, str) else k[0]).startswith("ps_")]
        if ps:
            writes = list(writes) + [k for k in ps if k not in writes]
        return reads, writes

    _cap = None
    _ns = None
    _atom = None
    SHARED_STR = ("ps_", "a_", "c_", "m_")
    SHARED_NAMES = {"gqk", "invA", "invB", "Wq", "Wkv", "Wab", "Wc", "QT", "KT", "Vd", "ps_pp", "ps_pq", "ps_pkv",
                    "QTs", "KTs", "BTs", "pr", "tab", "xt", "xn", "xnT", "junk", "ssx", "rx", "rx_t",
                    "acc", "hT", "Lall", "gates"}

    def _nskey(self, k):
        if self._ns is None:
            return k
        if isinstance(k, str):
            if k.startswith(self.SHARED_STR) or k in self.SHARED_NAMES:
                return k
            return (k, "ns", self._ns)
        if k[0] in self.SHARED_NAMES or k[0].startswith("ps_"):
            return k
        return tuple(k) + ("ns", self._ns)

    def begin_capture(self, ns):
        self._cap = []
        self._ns = ns
        self._atom = None

    def atom_begin(self):
        if self._cap is not None:
            self._atom = []

    def atom_end(self):
        if self._cap is not None and self._atom is not None:
            self._cap.append(self._atom)
            self._atom = None

    def _capture(self, item):
        if self._atom is not None:
            self._atom.append(item)
        else:
            self._cap.append([item])

    def end_capture(self):
        c = self._cap
        self._cap = None
        self._ns = None
        return c

    def replay(self, streams):
        idx = [0] * len(streams)
        live = True
        while live:
            live = False
            for i, st in enumerate(streams):
                if idx[i] < len(st):
                    unit = st[idx[i]]
                    idx[i] += 1
                    live = True
                    for kind, eng, fn, r, w in unit:
                        (self.op if kind == "op" else self.dma)(eng, fn, r, w)

    def op(self, eng, fn, reads=(), writes=()):
        if self._cap is not None:
            self._capture(("op", eng, fn, [self._nskey(k) for k in reads], [self._nskey(k) for k in writes]))
            return
        reads, writes = self._excl(reads, writes)
        waits = self._deps(eng, reads, writes)
        idx = self.done[eng] + len(self.ops[eng])
        self.ops[eng].append(_Op(fn, waits, None))
        self._record((eng, idx), reads, writes)
        return idx

    def dma(self, eng, fn, reads=(), writes=()):
        if self._cap is not None:
            self._capture(("dma", eng, fn, [self._nskey(k) for k in reads], [self._nskey(k) for k in writes]))
            return
        waits = self._deps(eng, reads, writes)
        if eng == "pool":
            j = self.n_dma - self.n_sw + self.sw_rr
            self.sw_rr = (self.sw_rr + 1) % self.n_sw
        else:
            j = self.dma_rr
            self.dma_rr = (j + 1) % (self.n_dma - self.n_sw)
        prev = self.dma_cnt[j]
        prod = ("dma", j)
        seen = self.seen[eng]
        if prev > 0 and seen.get(prod, -1) < prev:
            seen[prod] = prev
            waits.append((prod, prev))
        self.dma_cnt[j] = prev + 1
        self.ops[eng].append(_Op(fn, waits, (j, prev + 1)))
        self._record((prod, prev + 1), reads, writes)

    def barrier(self):
        last = {}
        for e in ENGS:
            if self.ops[e]:
                for i in range(len(self.ops[e]) - 1, -1, -1):
                    o = self.ops[e][i]
                    if o.dma is None and o.fn is not None:
                        o.signal = True
                        last[e] = self.done[e] + i
                        break
        for f in ENGS:
            waits = []
            seen = self.seen[f]
            for e, tick in last.items():
                if e != f and seen.get(e, -1) < tick:
                    waits.append((e, tick))
                    seen[e] = tick
            for j in range(self.n_dma):
                c = self.dma_cnt[j]
                prod = ("dma", j)
                if c > 0 and seen.get(prod, -1) < c:
                    waits.append((prod, c))
                    seen[prod] = c
            self.ops[f].append(_Op(None, waits, None))
        for f in ENGS:
            for e in ENGS:
                n = self.done[e] + len(self.ops[e]) - 1
                if e != f:
                    if e in last:
                        self.seen[f][e] = max(self.seen[f].get(e, -1), last[e])
        self.res = {}

    def flush(self, block, esem, dsem):
        engobj = {"pe": "tensor", "act": "scalar", "dve": "vector",
                  "pool": "gpsimd", "sp": "sync"}
        for e in ENGS:
            c = self.sigbase[e]
            for i, o in enumerate(self.ops[e]):
                if o.signal:
                    c += 1
                    self.sigmap[e][self.done[e] + i] = c

        def run(e):
            def body(eng):
                for o in self.ops[e]:
                    for prod, tick in o.waits:
                        if isinstance(prod, tuple):
                            eng.wait_ge(dsem[prod[1]], 16 * tick)
                        else:
                            eng.wait_ge(esem[prod], self.sigmap[prod][tick])
                    if o.fn is None:
                        continue
                    ins = o.fn(eng)
                    if o.dma is not None:
                        ins.then_inc(dsem[o.dma[0]], 16)
                    elif o.signal:
                        ins.then_inc(esem[e], 1)
            return body

        for e in ENGS:
            if self.ops[e]:
                getattr(block, engobj[e])(run(e))
        for e in ENGS:
            self.sigbase[e] = max([self.sigbase[e]] + [v for v in self.sigmap[e].values()])
            self.done[e] += len(self.ops[e])
            self.ops[e] = []


def bc_rows(ap, nparts=128):
    n = ap.shape[-1]
    return bass.AP(ap.tensor, ap.offset, [[0, nparts], [1, n]])


class Ctx:
    def __getattr__(self, name):
        if name in W_SHAPES:
            ap = self.nc.dram_tensor(name, W_SHAPES[name], F32, kind="ExternalInput").ap()
            self.__dict__[name] = ap
            self.used_inputs.append(name)
            return ap
        raise AttributeError(name)

    def tap(self, name, src_ap, shape, reads):
        if name not in self.taps:
            return
        d = self.nc.dram_tensor("tap_" + name, list(shape), F32, kind="ExternalOutput").ap()
        self.P.dma("sp", lambda e: e.dma_start(out=d, in_=src_ap), reads=reads, writes=["tap_" + name])


def moe_phase(K, layer, x_in, x_out, n_exp=16, stage=9, plevel=9):
    nc, P = K.nc, K.P
    SBT = 16
    NSB = NT // SBT
    with ExitStack() as es:
        def sb(name, shape, dt):
            return es.enter_context(nc.sbuf_tensor(name + "_L%d" % layer, shape, dt))

        def ps(name, shape, dt):
            return es.enter_context(nc.psum_tensor(name + "_L%d" % layer, shape, dt))

        acc = sb("m_acc", [128, SBT, D], F32)
        hT = sb("m_hT", [128, 8, SBT * 128], BF16)
        wgu = [sb("m_wgu%d" % i, [128, 8, 1024], BF16) for i in range(2)]
        wd = [sb("m_wd%d" % i, [128, 4, 1024], BF16) for i in range(2)]
        hid = [sb("m_hid%d" % i, [128, 4, 512], BF16) for i in range(2)]
        sg = [sb("m_sg%d" % i, [128, 512], F32) for i in range(2)]
        junks = [sb("m_junk%d" % i, [128, D], BF16) for i in range(2)]
        h32s = [sb("m_h32%d" % i, [128, D], F32) for i in range(2)]
        hT32s = [sb("m_hT32%d" % i, [128, 8, 128], F32) for i in range(2)]
        gain = sb("m_gain", [128, D], F32)
        wr = sb("m_wr", [128, 8, 20], F32)
        rb = sb("m_rb", [128, 20], F32)
        sss = [sb("m_ss%d" % i, [128, 4], F32) for i in range(2)]
        Lall = sb("m_L", [128, SBT, 20], F32)
        gates = sb("m_gates", [128, SBT, 16], F32)
        r_gmax = sb("r_gmax", [128, SBT], F32)
        r_gsh = sb("r_gsh", [128, SBT, 4], F32)
        r_gexp = sb("r_gexp", [128, SBT, 4], F32)
        r_gsum = sb("r_gsum", [128, SBT], F32)
        r_gp = sb("r_gp", [128, SBT], F32)
        r_pen = sb("r_pen", [128, SBT, 4], F32)
        r_em = sb("r_em", [128, SBT, 16], F32)
        r_m1 = sb("r_m1", [128, SBT], F32)
        r_mask1 = sb("r_mask1", [128, SBT, 16], F32)
        r_em2 = sb("r_em2", [128, SBT, 16], F32)
        r_m2 = sb("r_m2", [128, SBT], F32)
        r_mask2 = sb("r_mask2", [128, SBT, 16], F32)
        r_ed = sb("r_ed", [128, SBT], F32)
        r_w1 = sb("r_w1", [128, SBT], F32)
        r_w2 = sb("r_w2", [128, SBT], F32)

        ptr = ps("m_ptr", [128, 8, 128], F32)
        pG = [ps("m_pG%d" % i, [128, 512], F32) for i in range(2)]
        pU = [ps("m_pU%d" % i, [128, 512], F32) for i in range(2)]
        pY = [ps("m_pY%d" % i, [128, 512], F32) for i in range(2)]

        P.dma("sp", lambda e: e.dma_start(out=gain[:], in_=bc_rows(K.ffn_norm[layer:layer + 1, :])),
              writes=["m_gain"])
        P.dma("sp", lambda e: e.dma_start(out=wr[:, :, 0:4],
                                          in_=K.moe_w_group[layer].rearrange("(c p) n -> p c n", p=128)),
              writes=["m_wr"])
        P.dma("sp", lambda e: e.dma_start(out=wr[:, :, 4:20],
                                          in_=K.moe_w_router[layer].rearrange("(c p) n -> p c n", p=128)),
              writes=["m_wr"])
        P.dma("sp", lambda e: e.dma_start(out=rb[:, 0:4], in_=bc_rows(K.moe_b_group[layer:layer + 1, :])),
              writes=["m_rb"])
        P.dma("sp", lambda e: e.dma_start(out=rb[:, 4:20], in_=bc_rows(K.moe_b_router[layer:layer + 1, :])),
              writes=["m_rb"])

        def load_w(e_idx, buf):
            wg_src = K.moe_w_gate[layer, e_idx].rearrange("(c p) n -> p c n", p=128)
            wu_src = K.moe_w_up[layer, e_idx].rearrange("(c p) n -> p c n", p=128)
            wd_src = K.moe_w_down[layer, e_idx].rearrange("(c p) n -> p c n", p=128)
            for h in range(2):
                P.dma("pool", lambda e, h=h: e.dma_start(out=wgu[buf][:, 4 * h:4 * h + 4, 0:512],
                                                       in_=wg_src[:, 4 * h:4 * h + 4, :]),
                      writes=[("wgu", buf, 0, h)])
                P.dma("pool", lambda e, h=h: e.dma_start(out=wgu[buf][:, 4 * h:4 * h + 4, 512:1024],
                                                       in_=wu_src[:, 4 * h:4 * h + 4, :]),
                      writes=[("wgu", buf, 1, h)])
            P.dma("pool", lambda e: e.dma_start(out=wd[buf][:], in_=wd_src), writes=[("wd", buf)])

        pend = [None]
        nblk = [0]

        def emit_gu(ex, buf, b, n):
            hb = hid[n % 2]
            for f in range(4):
                i2 = f % 2
                for c in range(8):
                    P.op("pe", lambda e, c=c, f=f, i2=i2: e.matmul(
                        pG[i2][:], lhsT=wgu[buf][:, c, f * 128:(f + 1) * 128],
                        rhs=hT[:, c, b * 512:(b + 1) * 512], start=(c == 0), stop=(c == 7)),
                        reads=[("wgu", buf, 0, c // 4), ("hT", b)], writes=[("ps_G", i2)])
                for c in range(8):
                    P.op("pe", lambda e, c=c, f=f, i2=i2: e.matmul(
                        pU[i2][:], lhsT=wgu[buf][:, c, 512 + f * 128:512 + (f + 1) * 128],
                        rhs=hT[:, c, b * 512:(b + 1) * 512], start=(c == 0), stop=(c == 7)),
                        reads=[("wgu", buf, 1, c // 4), ("hT", b)], writes=[("ps_U", i2)])
                P.op("act", lambda e, i2=i2: e.activation(out=sg[i2][:], in_=pG[i2][:], func=AF.Silu),
                     reads=[("ps_G", i2)], writes=[("sg", i2)])
                P.op("dve", lambda e, i2=i2, f=f: e.tensor_tensor(out=hb[:, f, :], in0=sg[i2][:], in1=pU[i2][:], op=ALU.mult),
                     reads=[("sg", i2), ("ps_U", i2)], writes=[("hid", n % 2, f)])

        def emit_y(ex, buf, b, n):
            hb = hid[n % 2]
            for s4 in range(4):
                tl = b * 4 + s4
                for nh in range(2):
                    yb = (s4 * 2 + nh) % 2
                    for f in range(4):
                        P.op("pe", lambda e, f=f, s4=s4, nh=nh, yb=yb: e.matmul(
                            pY[yb][:], lhsT=hb[:, f, s4 * 128:(s4 + 1) * 128],
                            rhs=wd[buf][:, f, nh * 512:(nh + 1) * 512], start=(f == 0), stop=(f == 3)),
                            reads=[("hid", n % 2, f), ("wd", buf)], writes=["ps_Y%d" % yb])
                    P.op("dve", lambda e, tl=tl, nh=nh, yb=yb: e.scalar_tensor_tensor(
                        out=acc[:, tl, nh * 512:(nh + 1) * 512], in0=pY[yb][:],
                        scalar=gates[:, tl, ex:ex + 1], in1=acc[:, tl, nh * 512:(nh + 1) * 512],
                        op0=ALU.mult, op1=ALU.add),
                        reads=["ps_Y%d" % yb, "gates", ("acc", tl)], writes=[("acc", tl)])

        for sbi in range(NSB):
            def prep_tile(tl):
                t = sbi * SBT + tl
                junk = junks[tl % 2]
                h32 = h32s[tl % 2]
                hT32 = hT32s[tl % 2]
                ss = sss[tl % 2]
                P.dma("sp", lambda e, t=t, tl=tl: e.dma_start(out=acc[:, tl, :], in_=x_in[t * 128:(t + 1) * 128, :]),
                      writes=[("acc", tl)])
                if plevel < 1:
                    return
                P.op("act", lambda e, tl=tl: e.activation(out=junk[:], in_=acc[:, tl, :], func=AF.Square,
                                                          accum_out=ss[:, 0:1]),
                     reads=[("acc", tl)], writes=["junk", "ss0"])
                P.op("dve", lambda e: e.tensor_scalar(out=ss[:, 1:2], in0=ss[:, 0:1], scalar1=1.0 / D, scalar2=EPS,
                                                      op0=ALU.mult, op1=ALU.add), reads=["ss0"], writes=["ss1"])
                P.op("act", lambda e: e.activation(out=ss[:, 2:3], in_=ss[:, 1:2], func=AF.Sqrt),
                     reads=["ss1"], writes=["ss2"])
                P.op("dve", lambda e: e.reciprocal(out=ss[:, 3:4], in_=ss[:, 2:3]), reads=["ss2"], writes=["ss3"])
                if plevel < 2:
                    return
                P.op("dve", lambda e, tl=tl: e.scalar_tensor_tensor(out=h32[:], in0=acc[:, tl, :], scalar=ss[:, 3:4],
                                                                     in1=gain[:], op0=ALU.mult, op1=ALU.mult),
                     reads=[("acc", tl), "ss3", "m_gain"], writes=["h32"])
                if plevel < 3:
                    return
                P.atom_begin()
                for c in range(8):
                    P.op("pe", lambda e, c=c: e.transpose(out=ptr[:, c, :], in_=h32[:, c * 128:(c + 1) * 128],
                                                          identity=K.ident32[:]),
                         reads=["h32"], writes=["ps_ptr"])
                P.op("act", lambda e: e.copy(out=hT32[:], in_=ptr[:]), reads=["ps_ptr"], writes=["hT32"])
                P.op("dve", lambda e, tl=tl: e.tensor_copy(out=hT[:, :, tl * 128:(tl + 1) * 128], in_=ptr[:]),
                     reads=["ps_ptr"], writes=[("hT", tl // 4)])
                P.atom_end()
                if plevel < 4:
                    return
                P.atom_begin()
                for c in range(8):
                    P.op("pe", lambda e, c=c: e.matmul(pY[1][:, 0:20], lhsT=hT32[:, c, :], rhs=wr[:, c, :],
                                                       start=(c == 0), stop=(c == 7)),
                         reads=["hT32", "m_wr"], writes=["ps_Y1"])
                P.op("dve", lambda e, tl=tl: e.tensor_tensor(out=Lall[:, tl, :], in0=pY[1][:, 0:20], in1=rb[:],
                                                             op=ALU.add),
                     reads=["ps_Y1", "m_rb"], writes=["Lall"])
                P.atom_end()


            def cap_prep(tl, ns):
                P.begin_capture(ns)
                prep_tile(tl)
                return P.end_capture()

            for tp in range(SBT // 2):
                P.replay([cap_prep(2 * tp, 0), cap_prep(2 * tp + 1, 1)])

            if stage >= 1:
                LG = Lall[:, :, 0:4]
                LE = Lall[:, :, 4:20]

                def b3(ap2, n):
                    return ap2.unsqueeze(2).to_broadcast([128, SBT, n])

                P.op("dve", lambda e: e.tensor_reduce(out=r_gmax[:], in_=LG, axis=AX.X, op=ALU.max),
                     reads=["Lall"], writes=["r_gmax"])
                P.op("dve", lambda e: e.tensor_tensor(out=r_gsh[:], in0=LG, in1=b3(r_gmax[:], 4), op=ALU.subtract),
                     reads=["Lall", "r_gmax"], writes=["r_gsh"])
                P.op("act", lambda e: e.activation(out=r_gexp[:], in_=r_gsh[:], func=AF.Exp),
                     reads=["r_gsh"], writes=["r_gexp"])
                P.op("dve", lambda e: e.tensor_reduce(out=r_gsum[:], in_=r_gexp[:], axis=AX.X, op=ALU.add),
                     reads=["r_gexp"], writes=["r_gsum"])
                P.op("dve", lambda e: e.reciprocal(out=r_gp[:], in_=r_gsum[:]), reads=["r_gsum"], writes=["r_gp"])
                P.op("dve", lambda e: e.tensor_scalar(out=r_pen[:], in0=r_gsh[:], scalar1=0.0, scalar2=None,
                                                      op0=ALU.is_ge), reads=["r_gsh"], writes=["r_pen"])
                P.op("dve", lambda e: e.tensor_scalar(out=r_pen[:], in0=r_pen[:], scalar1=-NEG, scalar2=NEG,
                                                      op0=ALU.mult, op1=ALU.add), reads=["r_pen"], writes=["r_pen"])
                P.op("dve", lambda e: e.tensor_tensor(
                    out=r_em[:].rearrange("p t (g j) -> p t g j", g=4),
                    in0=LE.rearrange("p t (g j) -> p t g j", g=4),
                    in1=r_pen[:].unsqueeze(3).to_broadcast([128, SBT, 4, 4]), op=ALU.add),
                     reads=["Lall", "r_pen"], writes=["r_em"])
                P.op("dve", lambda e: e.tensor_reduce(out=r_m1[:], in_=r_em[:], axis=AX.X, op=ALU.max),
                     reads=["r_em"], writes=["r_m1"])
                P.op("dve", lambda e: e.tensor_tensor(out=r_em[:], in0=r_em[:], in1=b3(r_m1[:], 16), op=ALU.subtract),
                     reads=["r_em", "r_m1"], writes=["r_em"])
                P.op("dve", lambda e: e.tensor_scalar(out=r_mask1[:], in0=r_em[:], scalar1=0.0, scalar2=None,
                                                      op0=ALU.is_ge), reads=["r_em"], writes=["r_mask1"])
                P.op("dve", lambda e: e.scalar_tensor_tensor(out=r_em2[:], in0=r_mask1[:], scalar=NEG, in1=r_em[:],
                                                             op0=ALU.mult, op1=ALU.add),
                     reads=["r_mask1", "r_em"], writes=["r_em2"])
                P.op("dve", lambda e: e.tensor_reduce(out=r_m2[:], in_=r_em2[:], axis=AX.X, op=ALU.max),
                     reads=["r_em2"], writes=["r_m2"])
                P.op("dve", lambda e: e.tensor_tensor(out=r_mask2[:], in0=r_em2[:], in1=b3(r_m2[:], 16), op=ALU.is_ge),
                     reads=["r_em2", "r_m2"], writes=["r_mask2"])
                P.op("act", lambda e: e.activation(out=r_ed[:], in_=r_m2[:], func=AF.Exp),
                     reads=["r_m2"], writes=["r_ed"])
                P.op("dve", lambda e: e.tensor_scalar(out=r_ed[:], in0=r_ed[:], scalar1=1.0, scalar2=None, op0=ALU.add),
                     reads=["r_ed"], writes=["r_ed"])
                P.op("dve", lambda e: e.reciprocal(out=r_ed[:], in_=r_ed[:]), reads=["r_ed"], writes=["r_ed"])
                P.op("dve", lambda e: e.tensor_tensor(out=r_w1[:], in0=r_gp[:], in1=r_ed[:], op=ALU.mult),
                     reads=["r_gp", "r_ed"], writes=["r_w1"])
                P.op("dve", lambda e: e.tensor_tensor(out=r_w2[:], in0=r_gp[:], in1=r_w1[:], op=ALU.subtract),
                     reads=["r_gp", "r_w1"], writes=["r_w2"])
                P.op("dve", lambda e: e.tensor_tensor(out=r_mask1[:], in0=r_mask1[:], in1=b3(r_w1[:], 16), op=ALU.mult),
                     reads=["r_mask1", "r_w1"], writes=["r_mask1"])
                P.op("dve", lambda e: e.tensor_tensor(out=r_mask2[:], in0=r_mask2[:], in1=b3(r_w2[:], 16), op=ALU.mult),
                     reads=["r_mask2", "r_w2"], writes=["r_mask2"])
                P.op("dve", lambda e: e.tensor_tensor(out=gates[:], in0=r_mask1[:], in1=r_mask2[:], op=ALU.add),
                     reads=["r_mask1", "r_mask2"], writes=["gates"])

            if sbi == 0:
                K.tap('gates', gates[:], [128, SBT, 16], ['gates'])
                K.tap('Lall', Lall[:], [128, SBT, 20], ['Lall'])
            if stage >= 2 and sbi == 0:
                load_w(0, 0)
            for ex in range(n_exp if stage >= 2 else 0):
                buf = ex % 2
                for b in range(SBT // 4):
                    emit_gu(ex, buf, b, nblk[0])
                    if pend[0] is not None:
                        emit_y(*pend[0])
                    pend[0] = (ex, buf, b, nblk[0])
                    nblk[0] += 1
                    if b == 0:
                        if ex + 1 < n_exp:
                            load_w(ex + 1, 1 - buf)
                        elif sbi + 1 < NSB:
                            load_w(0, 1 - buf)
            if pend[0] is not None:
                emit_y(*pend[0])
                pend[0] = None
            for tl in range(SBT):
                t = sbi * SBT + tl
                P.dma("sp", lambda e, t=t, tl=tl: e.dma_start(out=x_out[t * 128:(t + 1) * 128, :], in_=acc[:, tl, :]),
                      reads=[("acc", tl)], writes=[("xout", t)])
        P.barrier()
        P.flush(K.block, K.esem, K.dsem)


def bc(ap, pos, n):
    return ap.unsqueeze(pos).to_broadcast(list(ap.shape[:pos]) + [n] + list(ap.shape[pos:]))


class Phase:
    def __init__(self, K):
        self.K = K
        self.es = ExitStack()
        self.P = K.P
        K.uid = getattr(K, "uid", 0) + 1
        self.sfx = "_u%d" % K.uid

    def __enter__(self):
        self.es.__enter__()
        return self

    def __exit__(self, *a):
        self.P.barrier()
        self.P.flush(self.K.block, self.K.esem, self.K.dsem)
        return self.es.__exit__(*a)

    def sb(self, name, shape, dt=F32):
        return self.es.enter_context(self.K.nc.sbuf_tensor(name + self.sfx, shape, dt))

    def ps(self, name, shape, dt=F32):
        return self.es.enter_context(self.K.nc.psum_tensor(name + self.sfx, shape, dt))

    def rstd(self, eng2, ss, out, invd, n, key_in, key_out, tmp):
        P = self.P
        if isinstance(invd, float):
            P.op("dve", lambda e: e.tensor_scalar(out=tmp, in0=ss, scalar1=invd, scalar2=EPS, op0=ALU.mult,
                                                  op1=ALU.add), reads=[key_in], writes=[key_out + "_t"])
        else:
            P.op("dve", lambda e: e.tensor_tensor(out=tmp, in0=ss, in1=invd, op=ALU.mult),
                 reads=[key_in], writes=[key_out + "_t"])
            P.op("dve", lambda e: e.tensor_scalar(out=tmp, in0=tmp, scalar1=EPS, scalar2=None, op0=ALU.add),
                 reads=[key_out + "_t"], writes=[key_out + "_t"])
        P.op("act", lambda e: e.activation(out=tmp, in_=tmp, func=AF.Sqrt), reads=[key_out + "_t"],
             writes=[key_out + "_t"])
        P.op("dve", lambda e: e.reciprocal(out=out, in_=tmp), reads=[key_out + "_t"], writes=[key_out])

    def rope(self, x1, x2, cos, sin, o1, o2, t, rkeys, wkey, eng="dve"):
        P = self.P
        k = [wkey + "_t%d" % i for i in range(4)]
        P.op(eng, lambda e: e.tensor_tensor(out=t[0], in0=x1, in1=cos, op=ALU.mult), reads=rkeys, writes=[k[0]])
        P.op(eng, lambda e: e.tensor_tensor(out=t[1], in0=x2, in1=sin, op=ALU.mult), reads=rkeys, writes=[k[1]])
        P.op(eng, lambda e: e.tensor_tensor(out=t[2], in0=x2, in1=cos, op=ALU.mult), reads=rkeys, writes=[k[2]])
        P.op(eng, lambda e: e.tensor_tensor(out=t[3], in0=x1, in1=sin, op=ALU.mult), reads=rkeys, writes=[k[3]])
        P.op(eng, lambda e: e.tensor_tensor(out=o1, in0=t[0], in1=t[1], op=ALU.subtract),
             reads=[k[0], k[1]], writes=[wkey + "_o1"])
        P.op(eng, lambda e: e.tensor_tensor(out=o2, in0=t[2], in1=t[3], op=ALU.add),
             reads=[k[2], k[3]], writes=[wkey + "_o2"])
        return [wkey + "_o1", wkey + "_o2"]


def load_bc(ph, name, src_row, n):
    t = ph.sb(name, [128, n], F32)
    ph.P.dma("sp", lambda e: e.dma_start(out=t[:], in_=bc_rows(src_row)), writes=[name])
    return t


def l0_prep(K, x_in):
    nc, P = K.nc, K.P
    QT, KT, Vd = K.QT, K.KT, K.Vd
    with Phase(K) as ph:
        sb, ps = ph.sb, ph.ps
        Wab = sb("a_Wab", [128, 8, 1184], BF16)
        Wq = sb("a_Wq", [128, 2, 768], BF16)
        Wkv = sb("a_Wkv", [128, 1024], BF16)
        P.dma("pool", lambda e: e.dma_start(out=Wab[:], in_=K.w_in_ab[0].rearrange("(c p) n -> p c n", p=128)), writes=["Wab"])
        P.dma("pool", lambda e: e.dma_start(out=Wq[:], in_=K.mla_w_q_up[0].rearrange("(c p) n -> p c n", p=128)), writes=["Wq"])
        P.dma("pool", lambda e: e.dma_start(out=Wkv[:], in_=K.mla_w_kv_up[0]), writes=["Wkv"])
        gmix = load_bc(ph, "a_gmix", K.mix_norm[0:1, :], 1024)
        gqa = load_bc(ph, "a_gqa", K.mla_q_a_norm[0:1, :], 256)
        gkva = load_bc(ph, "a_gkva", K.mla_kv_a_norm[0:1, :], 128)
        gqn = load_bc(ph, "a_gqn", K.mla_qn_gain[0:1, :], 64)
        gkn = load_bc(ph, "a_gkn", K.mla_kn_gain[0:1, :], 64)
        gqr = load_bc(ph, "a_gqr", K.mla_qr_gain[0:1, :], 32)
        gkr = load_bc(ph, "a_gkr", K.mla_kr_gain[0:1, :], 32)
        gbq = load_bc(ph, "a_gbq", K.gqa_q_gain[0:1, :], 64)
        gbk = load_bc(ph, "a_gbk", K.gqa_k_gain[0:1, :], 64)
        gqk = sb("a_gqk", [128, 10, 64], F32)
        P.op("dve", lambda e: e.tensor_copy(out=gqk[:, 0:8, :], in_=bc(gbq[:], 1, 8)), reads=["a_gbq"], writes=["gqk"])
        P.op("dve", lambda e: e.tensor_copy(out=gqk[:, 8:10, :], in_=bc(gbk[:], 1, 2)), reads=["a_gbk"], writes=["gqk"])
        invA = sb("a_invA", [128, 13], F32)
        invB = sb("a_invB", [128, 24], F32)
        P.op("dve", lambda e: e.memset(invA[:, 0:1], 1.0 / 256), writes=["invA"])
        P.op("dve", lambda e: e.memset(invA[:, 1:2], 1.0 / 128), writes=["invA"])
        P.op("dve", lambda e: e.memset(invA[:, 2:3], 1.0 / 32), writes=["invA"])
        P.op("dve", lambda e: e.memset(invA[:, 3:13], 1.0 / 64), writes=["invA"])
        P.op("dve", lambda e: e.memset(invB[:, 0:8], 1.0 / 64), writes=["invB"])
        P.op("dve", lambda e: e.memset(invB[:, 8:16], 1.0 / 32), writes=["invB"])
        P.op("dve", lambda e: e.memset(invB[:, 16:24], 1.0 / 64), writes=["invB"])

        xt = sb("a_xt", [128, D], F32)
        junk = sb("a_junk", [128, D], BF16)
        ssx = sb("a_ssx", [128, 4], F32)
        xn = sb("a_xn", [128, D], BF16)
        xnT = sb("a_xnT", [128, 8, 128], BF16)
        prs = [sb("a_pr%d" % i, [128, 1184], F32) for i in range(4)]
        tabs = [sb("a_tab%d" % i, [128, 96], F32) for i in range(4)]
        QTs = [sb("a_QTs%d" % i, [128, 8, 512], BF16) for i in range(2)]
        KTs = [sb("a_KTs%d" % i, [128, 8, 512], BF16) for i in range(2)]
        BTs = [sb("a_BTs%d" % i, [128, 5, 512], BF16) for i in range(2)]

        SBB = []
        for _i in range(2):
            _d = {}
            _d["sqA"] = sb("a_sqA_%d" % _i, [128, 1056], F32)
            _d["stA"] = sb("a_stA_%d" % _i, [128, 13], F32)
            _d["tA"] = sb("a_tA_%d" % _i, [128, 13], F32)
            _d["rA"] = sb("a_rA_%d" % _i, [128, 13], F32)
            _d["kr"] = sb("a_kr_%d" % _i, [128, 32], F32)
            _d["krot"] = sb("a_krot_%d" % _i, [128, 32], F32)
            _d["bn"] = sb("a_bn_%d" % _i, [128, 10, 64], F32)
            _d["q32"] = sb("a_q32_%d" % _i, [128, 768], F32)
            _d["kv32"] = sb("a_kv32_%d" % _i, [128, 1024], F32)
            _d["sqB"] = sb("a_sqB_%d" % _i, [128, 768], F32)
            _d["sqK"] = sb("a_sqK_%d" % _i, [128, 8, 64], F32)
            _d["stB"] = sb("a_stB_%d" % _i, [128, 24], F32)
            _d["tB"] = sb("a_tB_%d" % _i, [128, 24], F32)
            _d["rB"] = sb("a_rB_%d" % _i, [128, 24], F32)
            _d["tmpn"] = sb("a_tmpn_%d" % _i, [128, 8, 64], F32)
            _d["qr"] = sb("a_qr_%d" % _i, [128, 8, 32], F32)
            _d["lat"] = sb("a_lat_%d" % _i, [128, 384], BF16)
            _d["latT"] = sb("a_latT_%d" % _i, [128, 3, 128], BF16)
            _d["Btok"] = sb("a_Btok_%d" % _i, [128, 10, 64], BF16)
            _d["Vb"] = sb("a_Vb_%d" % _i, [128, 2, 64], BF16)
            _d["Qtok"] = sb("a_Qtok_%d" % _i, [128, 8, 96], BF16)
            _d["Ktok"] = sb("a_Ktok_%d" % _i, [128, 8, 96], BF16)
            _d["Vm"] = sb("a_Vm_%d" % _i, [128, 8, 64], BF16)
            _d["rtk"] = [sb("a_rtk%d_%d" % (j, _i), [128, 16], F32) for j in range(4)]
            _d["rtb"] = [sb("a_rtb%d_%d" % (j, _i), [128, 320], F32) for j in range(4)]
            _d["rtq"] = [sb("a_rtq%d_%d" % (j, _i), [128, 128], F32) for j in range(4)]
            SBB.append(_d)
        ptb = ps("a_ptb", [128, 8, 128], BF16)
        pp = [ps("a_pp%d" % i, [128, 512], F32) for i in range(3)]
        pq = ps("a_pq", [128, 2, 512], F32)
        pkv = ps("a_pkv", [128, 2, 512], F32)
        ident = K.identb

        def stageA(t):
            pb = t % 4
            pr = prs[pb]
            tab = tabs[pb]
            P.dma("sp", lambda e, t=t: e.dma_start(out=xt[:], in_=x_in[t * 128:(t + 1) * 128, :]), writes=["xt"])
            P.dma("sp", lambda e, t=t: e.dma_start(out=tab[:], in_=K.ropetab[t * 128:(t + 1) * 128, :]), writes=[("tab", pb)])
            P.op("act", lambda e: e.activation(out=junk[:], in_=xt[:], func=AF.Square, accum_out=ssx[:, 0:1]),
                 reads=["xt"], writes=["junk", "ssx"])
            ph.rstd("act", ssx[:, 0:1], ssx[:, 2:3], 1.0 / D, 1, "ssx", "rx", ssx[:, 1:2])
            P.op("dve", lambda e: e.scalar_tensor_tensor(out=xn[:], in0=xt[:], scalar=ssx[:, 2:3], in1=gmix[:],
                                                         op0=ALU.mult, op1=ALU.mult),
                 reads=["xt", "rx", "a_gmix"], writes=["xn"])
            P.atom_begin()
            for c in range(8):
                P.op("pe", lambda e, c=c: e.transpose(out=ptb[:, c, :], in_=xn[:, c * 128:(c + 1) * 128], identity=ident[:]),
                     reads=["xn"], writes=["ps_tb"])
            P.op("act", lambda e: e.copy(out=xnT[:], in_=ptb[:]), reads=["ps_tb"], writes=["xnT"])
            P.atom_end()
            groups = [(0, 512), (512, 1024), (1024, 1184)]
            for g, (n0, n1) in enumerate(groups):
                for c in range(8):
                    P.op("pe", lambda e, c=c, g=g, n0=n0, n1=n1: e.matmul(pp[g][:, 0:n1 - n0], lhsT=xnT[:, c, :],
                                                                         rhs=Wab[:, c, n0:n1], start=(c == 0), stop=(c == 7)),
                         reads=["xnT", "Wab"], writes=[("ps_pp", g)])
            P.op("act", lambda e: e.copy(out=pr[:, 0:512], in_=pp[0][:]), reads=[("ps_pp", 0)], writes=[("pr", pb, 0)])
            P.op("dve", lambda e: e.tensor_copy(out=pr[:, 512:1024], in_=pp[1][:]), reads=[("ps_pp", 1)], writes=[("pr", pb, 1)])
            P.op("act", lambda e: e.copy(out=pr[:, 1024:1184], in_=pp[2][:, 0:160]), reads=[("ps_pp", 2)], writes=[("pr", pb, 2)])

        def stageB(t):
            pb = t % 4
            _L = SBB[t % 2]
            sqA = _L["sqA"]
            stA = _L["stA"]
            tA = _L["tA"]
            rA = _L["rA"]
            kr = _L["kr"]
            krot = _L["krot"]
            bn = _L["bn"]
            q32 = _L["q32"]
            kv32 = _L["kv32"]
            sqB = _L["sqB"]
            sqK = _L["sqK"]
            stB = _L["stB"]
            tB = _L["tB"]
            rB = _L["rB"]
            tmpn = _L["tmpn"]
            qr = _L["qr"]
            lat = _L["lat"]
            latT = _L["latT"]
            Btok = _L["Btok"]
            Vb = _L["Vb"]
            Qtok = _L["Qtok"]
            Ktok = _L["Ktok"]
            Vm = _L["Vm"]
            rtk = _L["rtk"]
            rtb = _L["rtb"]
            rtq = _L["rtq"]
            pr = prs[pb]
            tab = tabs[pb]
            stg = (t // 4) % 2
            tq = t % 4
            c0 = tq * 128
            PR = [("pr", pb, 0), ("pr", pb, 1), ("pr", pb, 2)]
            P.op("pool", lambda e: e.tensor_tensor(out=sqA[:], in0=pr[:, 0:1056], in1=pr[:, 0:1056], op=ALU.mult),
                 reads=PR, writes=["sqA"])
            P.op("dve", lambda e: e.tensor_reduce(out=stA[:, 0:1], in_=sqA[:, 0:256], axis=AX.X, op=ALU.add), reads=["sqA"], writes=["stA0"])
            P.op("dve", lambda e: e.tensor_reduce(out=stA[:, 1:2], in_=sqA[:, 256:384], axis=AX.X, op=ALU.add), reads=["sqA"], writes=["stA1"])
            P.op("dve", lambda e: e.tensor_reduce(out=stA[:, 2:3], in_=sqA[:, 384:416], axis=AX.X, op=ALU.add), reads=["sqA"], writes=["stA2"])
            P.op("dve", lambda e: e.tensor_reduce(out=stA[:, 3:13], in_=sqA[:, 416:1056].rearrange("p (h d) -> p h d", d=64),
                                                  axis=AX.X, op=ALU.add), reads=["sqA"], writes=["stA3"])
            P.op("dve", lambda e: e.tensor_tensor(out=tA[:], in0=stA[:], in1=invA[:], op=ALU.mult),
                 reads=["stA0", "stA1", "stA2", "stA3", "invA"], writes=["tA"])
            P.op("dve", lambda e: e.tensor_scalar(out=tA[:], in0=tA[:], scalar1=EPS, scalar2=None, op0=ALU.add), reads=["tA"], writes=["tA"])
            P.op("act", lambda e: e.activation(out=tA[:], in_=tA[:], func=AF.Sqrt), reads=["tA"], writes=["tA"])
            P.op("dve", lambda e: e.reciprocal(out=rA[:], in_=tA[:]), reads=["tA"], writes=["rA"])
            P.op("dve", lambda e: e.scalar_tensor_tensor(out=lat[:, 0:256], in0=pr[:, 0:256], scalar=rA[:, 0:1], in1=gqa[:],
                                                         op0=ALU.mult, op1=ALU.mult), reads=PR + ["rA", "a_gqa"], writes=["lat0"])
            P.op("dve", lambda e: e.scalar_tensor_tensor(out=lat[:, 256:384], in0=pr[:, 256:384], scalar=rA[:, 1:2], in1=gkva[:],
                                                         op0=ALU.mult, op1=ALU.mult), reads=PR + ["rA", "a_gkva"], writes=["lat1"])
            P.atom_begin()
            for c in range(3):
                P.op("pe", lambda e, c=c: e.transpose(out=ptb[:, c, :], in_=lat[:, c * 128:(c + 1) * 128], identity=ident[:]),
                     reads=["lat0", "lat1"], writes=["ps_tb"])
            P.op("act", lambda e: e.copy(out=latT[:], in_=ptb[:, 0:3, :]), reads=["ps_tb"], writes=["latT"])
            for g in range(2):
                for c in range(2):
                    P.op("pe", lambda e, c=c, g=g: e.matmul(pq[:, g, 0:384], lhsT=latT[:, c, :], rhs=Wq[:, c, g * 384:(g + 1) * 384],
                                                          start=(c == 0), stop=(c == 1)), reads=["latT", "Wq"], writes=[("ps_pq", g)])
            for g in range(2):
                P.op("pe", lambda e, g=g: e.matmul(pkv[:, g, :], lhsT=latT[:, 2, :], rhs=Wkv[:, g * 512:(g + 1) * 512],
                                                   start=True, stop=True), reads=["latT", "Wkv"], writes=[("ps_pkv", g)])
            P.op("act", lambda e: e.copy(out=q32[:, 0:384], in_=pq[:, 0, 0:384]), reads=[("ps_pq", 0)], writes=[("q32", 0)])
            P.op("dve", lambda e: e.tensor_copy(out=q32[:, 384:768], in_=pq[:, 1, 0:384]), reads=[("ps_pq", 1)], writes=[("q32", 1)])
            P.op("act", lambda e: e.copy(out=kv32[:, 0:512], in_=pkv[:, 0, :]), reads=[("ps_pkv", 0)], writes=[("kv32", 0)])
            P.op("dve", lambda e: e.tensor_copy(out=kv32[:, 512:1024], in_=pkv[:, 1, :]), reads=[("ps_pkv", 1)], writes=[("kv32", 1)])
            P.atom_end()
            Q32 = [("q32", 0), ("q32", 1)]
            KV32 = [("kv32", 0), ("kv32", 1)]
            q32v = q32[:].rearrange("p (h d) -> p h d", d=96)
            kv32v = kv32[:].rearrange("p (h d) -> p h d", d=128)
            P.op("dve", lambda e: e.scalar_tensor_tensor(out=kr[:], in0=pr[:, 384:416], scalar=rA[:, 2:3], in1=gkr[:],
                                                         op0=ALU.mult, op1=ALU.mult), reads=PR + ["rA", "a_gkr"], writes=["kr"])
            kk = ph.rope(kr[:, 0:16], kr[:, 16:32], tab[:, 0:16], tab[:, 16:32], krot[:, 0:16], krot[:, 16:32],
                         [rtk[i][:] for i in range(4)], ["kr", ("tab", pb)], "krot")
            P.op("pool", lambda e: e.tensor_tensor(out=bn[:], in0=pr[:, 416:1056].rearrange("p (h d) -> p h d", d=64),
                                                   in1=bc(rA[:, 3:13], 2, 64), op=ALU.mult), reads=PR + ["rA"], writes=["bn"])
            P.op("pool", lambda e: e.tensor_tensor(out=bn[:], in0=bn[:], in1=gqk[:], op=ALU.mult), reads=["bn", "gqk"], writes=["bn"])
            bnv = bn[:].rearrange("p h (a b f) -> p h a b f", a=2, b=2)
            Bv = Btok[:].rearrange("p h (a b f) -> p h a b f", a=2, b=2)
            tabv = tab[:, 32:96].rearrange("p (a b f) -> p a b f", a=2, b=2)
            cosb = bc(tabv[:, :, 0, :], 1, 10)
            sinb = bc(tabv[:, :, 1, :], 1, 10)
            rtv = [rtb[i][:].rearrange("p (h a f) -> p h a f", h=10, a=2) for i in range(4)]
            bkeys = ph.rope(bnv[:, :, :, 0, :], bnv[:, :, :, 1, :], cosb, sinb, Bv[:, :, :, 0, :], Bv[:, :, :, 1, :],
                            rtv, ["bn", ("tab", pb)], "Btok", eng="pool")
            P.op("act", lambda e: e.copy(out=Vb[:], in_=pr[:, 1056:1184].rearrange("p (h d) -> p h d", d=64)),
                 reads=PR, writes=["Vb"])
            P.op("pool", lambda e: e.tensor_tensor(out=sqB[:], in0=q32[:], in1=q32[:], op=ALU.mult), reads=Q32, writes=["sqB"])
            P.op("pool", lambda e: e.tensor_tensor(out=sqK[:], in0=kv32v[:, :, 0:64], in1=kv32v[:, :, 0:64], op=ALU.mult),
                 reads=KV32, writes=["sqK"])
            sqBv = sqB[:].rearrange("p (h d) -> p h d", d=96)
            P.op("dve", lambda e: e.tensor_reduce(out=stB[:, 0:8], in_=sqBv[:, :, 0:64], axis=AX.X, op=ALU.add), reads=["sqB"], writes=["stB0"])
            P.op("dve", lambda e: e.tensor_reduce(out=stB[:, 8:16], in_=sqBv[:, :, 64:96], axis=AX.X, op=ALU.add), reads=["sqB"], writes=["stB1"])
            P.op("dve", lambda e: e.tensor_reduce(out=stB[:, 16:24], in_=sqK[:], axis=AX.X, op=ALU.add), reads=["sqK"], writes=["stB2"])
            P.op("dve", lambda e: e.tensor_tensor(out=tB[:], in0=stB[:], in1=invB[:], op=ALU.mult),
                 reads=["stB0", "stB1", "stB2", "invB"], writes=["tB"])
            P.op("dve", lambda e: e.tensor_scalar(out=tB[:], in0=tB[:], scalar1=EPS, scalar2=None, op0=ALU.add), reads=["tB"], writes=["tB"])
            P.op("act", lambda e: e.activation(out=tB[:], in_=tB[:], func=AF.Sqrt), reads=["tB"], writes=["tB"])
            P.op("dve", lambda e: e.reciprocal(out=rB[:], in_=tB[:]), reads=["tB"], writes=["rB"])
            P.op("dve", lambda e: e.tensor_tensor(out=tmpn[:], in0=q32v[:, :, 0:64], in1=bc(rB[:, 0:8], 2, 64), op=ALU.mult),
                 reads=Q32 + ["rB"], writes=["tmpn"])
            P.op("dve", lambda e: e.tensor_tensor(out=Qtok[:, :, 0:64], in0=tmpn[:], in1=bc(gqn[:], 1, 8), op=ALU.mult),
                 reads=["tmpn", "a_gqn"], writes=["Qtok_n"])
            P.op("dve", lambda e: e.tensor_tensor(out=qr[:], in0=q32v[:, :, 64:96], in1=bc(rB[:, 8:16], 2, 32), op=ALU.mult),
                 reads=Q32 + ["rB"], writes=["qr"])
            P.op("dve", lambda e: e.tensor_tensor(out=qr[:], in0=qr[:], in1=bc(gqr[:], 1, 8), op=ALU.mult),
                 reads=["qr", "a_gqr"], writes=["qr"])
            qrv = qr[:].rearrange("p h (b f) -> p h b f", b=2)
            Qrv = Qtok[:, :, 64:96].rearrange("p h (b f) -> p h b f", b=2)
            rt8 = [rtq[i][:].rearrange("p (h f) -> p h f", h=8) for i in range(4)]
            qkeys = ph.rope(qrv[:, :, 0, :], qrv[:, :, 1, :], bc(tab[:, 0:16], 1, 8), bc(tab[:, 16:32], 1, 8),
                            Qrv[:, :, 0, :], Qrv[:, :, 1, :], rt8, ["qr", ("tab", pb)], "Qtok_r")
            P.op("dve", lambda e: e.tensor_tensor(out=tmpn[:], in0=kv32v[:, :, 0:64], in1=bc(rB[:, 16:24], 2, 64), op=ALU.mult),
                 reads=KV32 + ["rB", "Qtok_n"], writes=["tmpn"])
            P.op("dve", lambda e: e.tensor_tensor(out=Ktok[:, :, 0:64], in0=tmpn[:], in1=bc(gkn[:], 1, 8), op=ALU.mult),
                 reads=["tmpn", "a_gkn"], writes=["Ktok_n"])
            P.op("act", lambda e: e.copy(out=Ktok[:, :, 64:96], in_=bc(krot[:], 1, 8)), reads=kk, writes=["Ktok_r"])
            P.op("act", lambda e: e.copy(out=Vm[:], in_=kv32v[:, :, 64:128]), reads=KV32, writes=["Vm"])
            P.atom_begin()
            for h in range(8):
                P.op("pe", lambda e, h=h: e.transpose(out=ptb[0:96, h, :], in_=Qtok[:, h, :], identity=ident[:]),
                     reads=["Qtok_n"] + qkeys, writes=["ps_tb"])
            P.op("dve", lambda e, stg=stg, c0=c0: e.tensor_copy(out=QTs[stg][0:96, :, c0:c0 + 128], in_=ptb[0:96, :, :]),
                 reads=["ps_tb"], writes=[("QTs", stg)])
            for h in range(8):
                P.op("pe", lambda e, h=h: e.transpose(out=ptb[0:96, h, :], in_=Ktok[:, h, :], identity=ident[:]),
                     reads=["Ktok_n", "Ktok_r"], writes=["ps_tb"])
            P.op("act", lambda e, stg=stg, c0=c0: e.copy(out=KTs[stg][0:96, :, c0:c0 + 128], in_=ptb[0:96, :, :]),
                 reads=["ps_tb"], writes=[("KTs", stg)])
            Bflat = Btok[:].rearrange("p h d -> p (h d)")
            for j in range(5):
                P.op("pe", lambda e, j=j: e.transpose(out=ptb[:, j, :], in_=Bflat[:, j * 128:(j + 1) * 128], identity=ident[:]),
                     reads=bkeys, writes=["ps_tb"])
            P.op("dve", lambda e, stg=stg, c0=c0: e.tensor_copy(out=BTs[stg][:, :, c0:c0 + 128], in_=ptb[:, 0:5, :]),
                 reads=["ps_tb"], writes=[("BTs", stg)])
            P.atom_end()
            P.dma("act", lambda e, t=t: e.dma_start(out=Vd[0:8, :, t, :].rearrange("h p d -> p h d"), in_=Vm[:]),
                  reads=["Vm"], writes=[("Vd", t)])
            P.dma("act", lambda e, t=t: e.dma_start(out=Vd[8:10, :, t, :].rearrange("h p d -> p h d"), in_=Vb[:]),
                  reads=["Vb"], writes=[("Vd", t)])
            if tq == 3:
                n0 = (t // 4) * 512
                P.dma("act", lambda e, stg=stg, n0=n0: e.dma_start(out=QT[0:8, :, n0:n0 + 512].rearrange("h d n -> d h n"),
                                                                  in_=QTs[stg][0:96, :, :]), reads=[("QTs", stg)], writes=["QT"])
                P.dma("act", lambda e, stg=stg, n0=n0: e.dma_start(out=KT[0:8, :, n0:n0 + 512].rearrange("h d n -> d h n"),
                                                                  in_=KTs[stg][0:96, :, :]), reads=[("KTs", stg)], writes=["KT"])
                for u in range(2):
                    P.dma("act", lambda e, stg=stg, n0=n0, u=u: e.dma_start(
                        out=QT[8 + u:16:2, 0:64, n0:n0 + 512].rearrange("h d n -> d h n"),
                        in_=BTs[stg][u * 64:(u + 1) * 64, 0:4, :]), reads=[("BTs", stg)], writes=["QT"])
                    P.dma("act", lambda e, stg=stg, n0=n0, u=u: e.dma_start(
                        out=KT[8 + u, 0:64, n0:n0 + 512], in_=BTs[stg][u * 64:(u + 1) * 64, 4, :]),
                        reads=[("BTs", stg)], writes=["KT"])


        def cap(fn, t, ns):
            P.begin_capture(ns)
            fn(t)
            return P.end_capture()

        stageA(0)
        stageA(1)
        for p in range(NT // 2):
            streams = [cap(stageB, 2 * p, 0), cap(stageB, 2 * p + 1, 1)]
            if 2 * p + 2 < NT:
                streams.append(cap(stageA, 2 * p + 2, None) + cap(stageA, 2 * p + 3, None))
            P.replay(streams)

def attn_full(K, heads):
    nc, P = K.nc, K.P
    QT, KT, Vd, OT = K.QT, K.KT, K.Vd, K.OT
    QB = 1024
    with Phase(K) as ph:
        sb, ps = ph.sb, ph.ps
        kT = [sb("f_kT%d" % i, [96, S], BF16) for i in range(2)]
        qT = [sb("f_qT%d" % i, [96, S], BF16) for i in range(2)]
        vs = [sb("f_v%d" % i, [128, NT, 128], BF16) for i in range(2)]
        vst = [sb("f_vst%d" % i, [128, NT, 64], BF16) for i in range(2)]
        pT = [sb("f_pT%d" % i, [128, QB], BF16) for i in range(3)]
        osb = [sb("f_osb%d" % i, [128, 512], F32) for i in range(4)]
        rec = sb("f_rec", [64, 512], F32)
        oTs = [sb("f_oTs%d" % i, [64, 512], BF16) for i in range(2)]
        sc = [ps("f_sc%d" % i, [128, QB], F32) for i in range(2)]
        oacc = [ps("f_oacc%d" % i, [128, 512], F32) for i in range(2)]
        for i in range(2):
            P.op("dve", lambda e, i=i: e.memset(vs[i][:, :, 64:128], 1.0), writes=[("vone", i)])
        iters = []
        for hi, (hq, kv, d, scale, orow) in enumerate(heads):
            for qb in range(S // QB):
                for kc in range(NT):
                    iters.append((hi, hq, kv, d, scale, orow, qb, kc))

        def load_head(hi):
            hq, kv, d, scale, orow = heads[hi]
            b = hi % 2
            P.dma("sp", lambda e: e.dma_start(out=qT[b][0:d, :], in_=QT[hq, 0:d, :]), reads=["QT"], writes=[("qT", b)])
            P.dma("sp", lambda e: e.dma_start(out=kT[b][0:d, :], in_=KT[kv, 0:d, :]), reads=["KT"], writes=[("kT", b)])
            P.dma("sp", lambda e: e.dma_start(out=vst[b][:], in_=Vd[kv]), reads=["Vd"], writes=[("vst", b)])
            P.op("dve", lambda e: e.tensor_copy(out=vs[b][:, :, 0:64], in_=vst[b][:]), reads=[("vst", b)], writes=[("vs", b)])

        def emit_qk(n):
            hi, hq, kv, d, scale, orow, qb, kc = iters[n]
            b = hi % 2
            si = n % 2
            pi = n % 3
            for hf in range(2):
                q0 = qb * QB + hf * 512
                P.op("pe", lambda e, hf=hf, q0=q0: e.matmul(
                    sc[si][:, hf * 512:(hf + 1) * 512], lhsT=kT[b][0:d, kc * 128:(kc + 1) * 128],
                    rhs=qT[b][0:d, q0:q0 + 512], start=True, stop=True),
                    reads=[("kT", b), ("qT", b)], writes=[("ps_sc", si)])
            P.op("act", lambda e: e.activation(out=pT[pi][:], in_=sc[si][:], func=AF.Exp, scale=scale),
                 reads=[("ps_sc", si)], writes=[("pT", pi)])

        fin = [0]

        def emit_pv(n):
            hi, hq, kv, d, scale, orow, qb, kc = iters[n]
            b = hi % 2
            pi = n % 3
            for hf in range(2):
                P.op("pe", lambda e, hf=hf: e.matmul(
                    oacc[hf][:, :], lhsT=vs[b][:, kc, :], rhs=pT[pi][:, hf * 512:(hf + 1) * 512],
                    start=(kc == 0), stop=(kc == NT - 1)),
                    reads=[("vs", b), ("vone", b), ("pT", pi)], writes=[("ps_oacc", hf)])
            if kc != NT - 1:
                return
            obs = []
            for hf in range(2):
                ob = fin[0] % 4
                fin[0] += 1
                obs.append(ob)
                P.op("dve", lambda e, hf=hf, ob=ob: e.tensor_copy(out=osb[ob][:], in_=oacc[hf][:]),
                     reads=[("ps_oacc", hf)], writes=[("osb", ob)])
            for hf in range(2):
                q0 = qb * QB + hf * 512
                ob = obs[hf]
                o2 = ob % 2
                P.op("dve", lambda e, ob=ob: e.reciprocal(out=rec[:], in_=osb[ob][64:128, :]), reads=[("osb", ob)], writes=["rec"])
                P.op("dve", lambda e, ob=ob, o2=o2: e.tensor_tensor(out=oTs[o2][:], in0=osb[ob][0:64, :], in1=rec[:], op=ALU.mult),
                     reads=[("osb", ob), "rec"], writes=[("oTs", o2)])
                P.dma("sp", lambda e, o2=o2, q0=q0: e.dma_start(out=OT[orow:orow + 64, q0:q0 + 512], in_=oTs[o2][:]),
                      reads=[("oTs", o2)], writes=["OT"])

        N = len(iters)
        load_head(0)
        if len(heads) > 1:
            load_head(1)
        emit_qk(0)
        for n in range(N):
            if n + 1 < N:
                if iters[n + 1][0] != iters[n][0] and iters[n + 1][0] + 1 < len(heads):
                    pass
                emit_qk(n + 1)
            emit_pv(n)
            if n + 1 < N and iters[n + 1][0] != iters[n][0] and iters[n][0] + 2 < len(heads):
                load_head(iters[n][0] + 2)


def out_proj(K, w_dram, x_in, x_out):
    nc, P = K.nc, K.P
    OT = K.OT
    with Phase(K) as ph:
        sb, ps = ph.sb, ph.ps
        Wo = sb("o_W", [128, 8, 1024], BF16)
        P.dma("pool", lambda e: e.dma_start(out=Wo[:], in_=w_dram.rearrange("(c p) n -> p c n", p=128)), writes=["Wo"])
        oT = [sb("o_oT%d" % i, [128, 8, 512], BF16) for i in range(2)]
        xt = [sb("o_xt%d" % i, [128, D], F32) for i in range(4)]
        po = [ps("o_po%d" % i, [128, 512], F32) for i in range(4)]
        for blk in range(S // 512):
            ob = blk % 2
            P.dma("sp", lambda e, ob=ob, blk=blk: e.dma_start(
                out=oT[ob][:], in_=OT[:, blk * 512:(blk + 1) * 512].rearrange("(c p) n -> p c n", p=128)),
                reads=["OT"], writes=[("oT", ob)])
            for s4 in range(4):
                t = blk * 4 + s4
                xb = t % 4
                P.dma("sp", lambda e, xb=xb, t=t: e.dma_start(out=xt[xb][:], in_=x_in[t * 128:(t + 1) * 128, :]), writes=[("xt", xb)])
                for nh in range(2):
                    pb = (t * 2 + nh) % 4
                    for c in range(8):
                        P.op("pe", lambda e, c=c, ob=ob, s4=s4, nh=nh, pb=pb: e.matmul(
                            po[pb][:], lhsT=oT[ob][:, c, s4 * 128:(s4 + 1) * 128], rhs=Wo[:, c, nh * 512:(nh + 1) * 512],
                            start=(c == 0), stop=(c == 7)), reads=[("oT", ob), "Wo"], writes=[("ps_po", pb)])
                    P.op("dve", lambda e, xb=xb, nh=nh, pb=pb: e.tensor_tensor(
                        out=xt[xb][:, nh * 512:(nh + 1) * 512], in0=po[pb][:], in1=xt[xb][:, nh * 512:(nh + 1) * 512], op=ALU.add),
                        reads=[("ps_po", pb), ("xt", xb)], writes=[("xt", xb)])
                P.dma("act", lambda e, xb=xb, t=t: e.dma_start(out=x_out[t * 128:(t + 1) * 128, :], in_=xt[xb][:]),
                      reads=[("xt", xb)], writes=[("xout", t)])


def l1_prep(K, x_in):
    nc, P = K.nc, K.P
    QT, KT, Vd = K.QT, K.KT, K.Vd
    with Phase(K) as ph:
        sb, ps = ph.sb, ph.ps
        Wc = sb("c_Wc", [128, 8, 1536], BF16)
        P.dma("pool", lambda e: e.dma_start(out=Wc[:], in_=K.w_in_c[0].rearrange("(c p) n -> p c n", p=128)), writes=["Wc"])
        gmix = load_bc(ph, "c_gmix", K.mix_norm[1:2, :], 1024)
        gq = load_bc(ph, "c_gq", K.win_q_gain[0:1, :], 64)
        gk = load_bc(ph, "c_gk", K.win_k_gain[0:1, :], 64)
        gqk = sb("c_gqk", [128, 20, 64], F32)
        P.op("dve", lambda e: e.tensor_copy(out=gqk[:, 0:16, :], in_=bc(gq[:], 1, 16)), reads=["c_gq"], writes=["gqk"])
        P.op("dve", lambda e: e.tensor_copy(out=gqk[:, 16:20, :], in_=bc(gk[:], 1, 4)), reads=["c_gk"], writes=["gqk"])
        xt = sb("c_xt", [128, D], F32)
        junk = sb("c_junk", [128, D], BF16)
        ssx = sb("c_ssx", [128, 4], F32)
        xn = sb("c_xn", [128, D], BF16)
        xnT = sb("c_xnT", [128, 8, 128], BF16)
        prs = [sb("c_pr%d" % i, [128, 1536], F32) for i in range(4)]
        BTs = [sb("c_BTs%d" % i, [128, 10, 512], BF16) for i in range(2)]
        SBB = []
        for _i in range(2):
            _d = {}
            _d["sq"] = sb("c_sq_%d" % _i, [128, 1280], F32)
            _d["st"] = sb("c_st_%d" % _i, [128, 20], F32)
            _d["tt"] = sb("c_tt_%d" % _i, [128, 20], F32)
            _d["rr"] = sb("c_rr_%d" % _i, [128, 20], F32)
            _d["bn"] = sb("c_bn_%d" % _i, [128, 20, 64], F32)
            _d["Btok"] = sb("c_Btok_%d" % _i, [128, 20, 64], BF16)
            _d["Vc"] = sb("c_Vc_%d" % _i, [128, 4, 64], BF16)
            SBB.append(_d)
        ptb = ps("c_ptb", [128, 8, 128], BF16)
        ptb2 = ps("c_ptb2", [128, 8, 128], BF16)
        pp = [ps("c_pp%d" % i, [128, 512], F32) for i in range(3)]
        ident = K.identb
        def stageA(t):
            pb = t % 4
            pr = prs[pb]
            P.dma("sp", lambda e, t=t: e.dma_start(out=xt[:], in_=x_in[t * 128:(t + 1) * 128, :]), writes=["xt"])
            P.op("act", lambda e: e.activation(out=junk[:], in_=xt[:], func=AF.Square, accum_out=ssx[:, 0:1]),
                 reads=["xt"], writes=["junk", "ssx"])
            ph.rstd("act", ssx[:, 0:1], ssx[:, 2:3], 1.0 / D, 1, "ssx", "rx", ssx[:, 1:2])
            P.op("dve", lambda e: e.scalar_tensor_tensor(out=xn[:], in0=xt[:], scalar=ssx[:, 2:3], in1=gmix[:],
                                                         op0=ALU.mult, op1=ALU.mult), reads=["xt", "rx", "c_gmix"], writes=["xn"])
            P.atom_begin()
            for c in range(8):
                P.op("pe", lambda e, c=c: e.transpose(out=ptb[:, c, :], in_=xn[:, c * 128:(c + 1) * 128], identity=ident[:]),
                     reads=["xn"], writes=["ps_tb"])
            P.op("act", lambda e: e.copy(out=xnT[:], in_=ptb[:]), reads=["ps_tb"], writes=["xnT"])
            P.atom_end()
            for g in range(3):
                for c in range(8):
                    P.op("pe", lambda e, c=c, g=g: e.matmul(pp[g][:], lhsT=xnT[:, c, :], rhs=Wc[:, c, g * 512:(g + 1) * 512],
                                                          start=(c == 0), stop=(c == 7)), reads=["xnT", "Wc"], writes=[("ps_pp", g)])
            P.op("act", lambda e: e.copy(out=pr[:, 0:512], in_=pp[0][:]), reads=[("ps_pp", 0)], writes=[("pr", pb, 0)])
            P.op("dve", lambda e: e.tensor_copy(out=pr[:, 512:1024], in_=pp[1][:]), reads=[("ps_pp", 1)], writes=[("pr", pb, 1)])
            P.op("act", lambda e: e.copy(out=pr[:, 1024:1536], in_=pp[2][:]), reads=[("ps_pp", 2)], writes=[("pr", pb, 2)])

        def stageB(t):
            pb = t % 4
            _L = SBB[t % 2]
            sq = _L["sq"]
            st = _L["st"]
            tt = _L["tt"]
            rr = _L["rr"]
            bn = _L["bn"]
            Btok = _L["Btok"]
            Vc = _L["Vc"]
            pr = prs[pb]
            stg = (t // 4) % 2
            c0 = (t % 4) * 128
            PR = [("pr", pb, 0), ("pr", pb, 1), ("pr", pb, 2)]
            prv = pr[:, 0:1280].rearrange("p (h d) -> p h d", d=64)
            P.op("pool", lambda e: e.tensor_tensor(out=sq[:], in0=pr[:, 0:1280], in1=pr[:, 0:1280], op=ALU.mult), reads=PR, writes=["sq"])
            P.op("dve", lambda e: e.tensor_reduce(out=st[:], in_=sq[:].rearrange("p (h d) -> p h d", d=64), axis=AX.X, op=ALU.add),
                 reads=["sq"], writes=["st"])
            ph.rstd("act", st[:], rr[:], 1.0 / 64, 20, "st", "rr", tt[:])
            P.op("dve", lambda e: e.tensor_tensor(out=bn[:], in0=prv, in1=bc(rr[:], 2, 64), op=ALU.mult), reads=PR + ["rr"], writes=["bn"])
            P.op("pool", lambda e: e.tensor_tensor(out=Btok[:], in0=bn[:], in1=gqk[:], op=ALU.mult), reads=["bn", "gqk"], writes=["Btok"])
            P.op("act", lambda e: e.copy(out=Vc[:], in_=pr[:, 1280:1536].rearrange("p (h d) -> p h d", d=64)), reads=PR, writes=["Vc"])
            Bflat = Btok[:].rearrange("p h d -> p (h d)")
            P.atom_begin()
            for j in range(8):
                P.op("pe", lambda e, j=j: e.transpose(out=ptb[:, j, :], in_=Bflat[:, j * 128:(j + 1) * 128], identity=ident[:]),
                     reads=["Btok"], writes=["ps_tb"])
            for j in range(2):
                P.op("pe", lambda e, j=j: e.transpose(out=ptb2[:, j, :], in_=Bflat[:, (8 + j) * 128:(9 + j) * 128], identity=ident[:]),
                     reads=["Btok"], writes=["ps_tb2"])
            P.op("dve", lambda e, stg=stg, c0=c0: e.tensor_copy(out=BTs[stg][:, 0:8, c0:c0 + 128], in_=ptb[:]),
                 reads=["ps_tb"], writes=[("BTs", stg)])
            P.op("act", lambda e, stg=stg, c0=c0: e.copy(out=BTs[stg][:, 8:10, c0:c0 + 128], in_=ptb2[:, 0:2, :]),
                 reads=["ps_tb2"], writes=[("BTs", stg)])
            P.atom_end()
            P.dma("act", lambda e, t=t: e.dma_start(out=Vd[0:4, :, t, :].rearrange("h p d -> p h d"), in_=Vc[:]),
                  reads=["Vc"], writes=[("Vd", t)])
            if t % 4 == 3:
                n0 = (t // 4) * 512
                for u in range(2):
                    P.dma("act", lambda e, stg=stg, n0=n0, u=u: e.dma_start(
                        out=QT[u:16:2, 0:64, n0:n0 + 512].rearrange("h d n -> d h n"),
                        in_=BTs[stg][u * 64:(u + 1) * 64, 0:8, :]), reads=[("BTs", stg)], writes=["QT"])
                    P.dma("act", lambda e, stg=stg, n0=n0, u=u: e.dma_start(
                        out=KT[u:4:2, 0:64, n0:n0 + 512].rearrange("h d n -> d h n"),
                        in_=BTs[stg][u * 64:(u + 1) * 64, 8:10, :]), reads=[("BTs", stg)], writes=["KT"])


        def cap(fn, t, ns):
            P.begin_capture(ns)
            fn(t)
            return P.end_capture()

        stageA(0)
        stageA(1)
        for p in range(NT // 2):
            streams = [cap(stageB, 2 * p, 0), cap(stageB, 2 * p + 1, 1)]
            if 2 * p + 2 < NT:
                streams.append(cap(stageA, 2 * p + 2, None) + cap(stageA, 2 * p + 3, None))
            P.replay(streams)

def attn_win(K):
    nc, P = K.nc, K.P
    QT, KT, Vd, OT = K.QT, K.KT, K.Vd, K.OT
    scale = 64 ** -0.5
    with Phase(K) as ph:
        sb, ps = ph.sb, ph.ps
        kT = [sb("w_kT%d" % i, [64, S], BF16) for i in range(2)]
        qT = [sb("w_qT%d" % i, [64, S], BF16) for i in range(2)]
        vs = [sb("w_v%d" % i, [128, NT, 128], BF16) for i in range(2)]
        vst = [sb("w_vst%d" % i, [128, NT, 64], BF16) for i in range(2)]
        bm = [sb("w_bm%d" % i, [128, 3, 128], F32) for i in range(2)]
        mask = sb("w_mask", [128, 3, 128], F32)
        ssb = [sb("w_ssb%d" % i, [128, 4, 3, 128], F32) for i in range(2)]
        pT = [sb("w_pT%d" % i, [128, 4, 3, 128], BF16) for i in range(2)]
        esink = sb("w_esink", [128, 16], F32)
        osb = [sb("w_osb%d" % i, [128, 512], F32) for i in range(2)]
        lnd = sb("w_lnd", [64, 512], F32)
        rec = sb("w_rec", [64, 512], F32)
        oTs = [sb("w_oTs%d" % i, [64, 512], BF16) for i in range(2)]
        scw = [ps("w_sc%d" % i, [128, 4, 3, 128], F32) for i in range(2)]
        oacc = [ps("w_oacc%d" % i, [128, 512], F32) for i in range(2)]
        for i in range(2):
            P.op("dve", lambda e, i=i: e.memset(vs[i][:, :, 64:128], 1.0), writes=[("vone", i)])
            P.op("dve", lambda e, i=i: e.memset(ssb[i][:], 0.0), writes=[("ssb", i)])
        P.dma("sp", lambda e: e.dma_start(out=mask[:], in_=K.winmask[:, :, :]), writes=["mask"])
        P.dma("sp", lambda e: e.dma_start(out=esink[:], in_=bc_rows(K.win_sink[0:1, :])), writes=["esink"])
        P.op("act", lambda e: e.activation(out=esink[:], in_=esink[:], func=AF.Exp), reads=["esink"], writes=["esink"])

        def load_head(h):
            b = h % 2
            kv = h // 4
            P.dma("sp", lambda e: e.dma_start(out=qT[b][:, :], in_=QT[h, 0:64, :]), reads=["QT"], writes=[("qT", b)])
            P.dma("sp", lambda e: e.dma_start(out=kT[b][:, :], in_=KT[kv, 0:64, :]), reads=["KT"], writes=[("kT", b)])
            P.dma("sp", lambda e: e.dma_start(out=vst[b][:], in_=Vd[kv]), reads=["Vd"], writes=[("vst", b)])
            P.op("dve", lambda e: e.tensor_copy(out=vs[b][:, :, 0:64], in_=vst[b][:]), reads=[("vst", b)], writes=[("vs", b)])
            P.dma("sp", lambda e: e.dma_start(out=bm[b][:], in_=K.winbias[h]), writes=[("bm", b)])
            P.op("dve", lambda e: e.tensor_tensor(out=bm[b][:], in0=bm[b][:], in1=mask[:], op=ALU.add),
                 reads=[("bm", b), "mask"], writes=[("bm", b)])

        groups = [(h, g) for h in range(16) for g in range(8)]

        def valid_of(g):
            valid = {}
            for blk in range(4):
                i = g * 4 + blk
                for j in range(3):
                    kc = i - 1 + j
                    if 0 <= kc < NT:
                        valid[(blk, j)] = kc
            return valid

        def emit_qk(n):
            h, g = groups[n]
            b = h % 2
            si = n % 2
            valid = valid_of(g)
            for (blk, j), kc in valid.items():
                i = g * 4 + blk
                P.op("pe", lambda e, blk=blk, j=j, kc=kc, i=i: e.matmul(
                    scw[si][:, blk, j, :], lhsT=kT[b][:, kc * 128:(kc + 1) * 128], rhs=qT[b][:, i * 128:(i + 1) * 128],
                    start=True, stop=True), reads=[("kT", b), ("qT", b)], writes=[("ps_scw", si)])
            if len(valid) == 12:
                P.op("dve", lambda e: e.scalar_tensor_tensor(
                    out=ssb[si][:], in0=scw[si][:], scalar=scale, in1=bc(bm[b][:], 1, 4), op0=ALU.mult, op1=ALU.add),
                    reads=[("ps_scw", si), ("bm", b)], writes=[("ssb", si)])
            else:
                for blk in range(4):
                    js = [j for j in range(3) if (blk, j) in valid]
                    j0, j1 = js[0], js[-1] + 1
                    P.op("dve", lambda e, blk=blk, j0=j0, j1=j1: e.scalar_tensor_tensor(
                        out=ssb[si][:, blk, j0:j1, :], in0=scw[si][:, blk, j0:j1, :], scalar=scale,
                        in1=bm[b][:, j0:j1, :], op0=ALU.mult, op1=ALU.add),
                        reads=[("ps_scw", si), ("bm", b)], writes=[("ssb", si)])
            P.op("act", lambda e: e.activation(out=pT[si][:], in_=ssb[si][:], func=AF.Exp),
                 reads=[("ssb", si)], writes=[("pT", si)])

        def emit_pv(n):
            h, g = groups[n]
            b = h % 2
            si = n % 2
            ob = n % 2
            valid = valid_of(g)
            for blk in range(4):
                js = [j for j in range(3) if (blk, j) in valid]
                for j in js:
                    kc = valid[(blk, j)]
                    P.op("pe", lambda e, blk=blk, j=j, kc=kc, js=js: e.matmul(
                        oacc[ob][:, blk * 128:(blk + 1) * 128], lhsT=vs[b][:, kc, :], rhs=pT[si][:, blk, j, :],
                        start=(j == js[0]), stop=(j == js[-1])),
                        reads=[("vs", b), ("vone", b), ("pT", si)], writes=[("ps_oacc", ob)])
            P.op("dve", lambda e: e.tensor_copy(out=osb[ob][:], in_=oacc[ob][:]), reads=[("ps_oacc", ob)], writes=[("osb", ob)])
            P.op("act", lambda e: e.activation(out=lnd[:], in_=osb[ob][64:128, :], func=AF.Ln, bias=esink[64:128, h:h + 1]),
                 reads=[("osb", ob), "esink"], writes=["lnd"])
            P.op("act", lambda e: e.activation(out=rec[:], in_=lnd[:], func=AF.Exp, scale=-1.0), reads=["lnd"], writes=["rec"])
            P.op("dve", lambda e: e.tensor_tensor(out=oTs[ob][:], in0=osb[ob][0:64, :], in1=rec[:], op=ALU.mult),
                 reads=[("osb", ob), "rec"], writes=[("oTs", ob)])
            P.dma("sp", lambda e: e.dma_start(out=OT[h * 64:(h + 1) * 64, g * 512:(g + 1) * 512], in_=oTs[ob][:]),
                  reads=[("oTs", ob)], writes=["OT"])

        N = len(groups)
        load_head(0)
        load_head(1)
        emit_qk(0)
        for n in range(N):
            if n + 1 < N:
                emit_qk(n + 1)
            emit_pv(n)
            if n + 1 < N and groups[n + 1][0] != groups[n][0] and groups[n][0] + 2 < 16:
                load_head(groups[n][0] + 2)


W_NAMES = ["mix_norm", "ffn_norm", "w_in_ab", "mla_q_a_norm", "mla_w_q_up", "mla_kv_a_norm",
           "mla_w_kv_up", "mla_qn_gain", "mla_kn_gain", "mla_qr_gain", "mla_kr_gain",
           "gqa_q_gain", "gqa_k_gain", "w_out_ab", "w_in_c", "win_q_gain", "win_k_gain",
           "win_sink", "w_out_c", "rel_bias", "moe_w_group", "moe_b_group", "moe_w_router",
           "moe_b_router", "moe_w_gate", "moe_w_up", "moe_w_down"]

W_SHAPES = {
    "mix_norm": [2, 1024], "ffn_norm": [2, 1024], "w_in_ab": [1, 1024, 1184], "mla_q_a_norm": [1, 256],
    "mla_w_q_up": [1, 256, 768], "mla_kv_a_norm": [1, 128], "mla_w_kv_up": [1, 128, 1024],
    "mla_qn_gain": [1, 64], "mla_kn_gain": [1, 64], "mla_qr_gain": [1, 32], "mla_kr_gain": [1, 32],
    "gqa_q_gain": [1, 64], "gqa_k_gain": [1, 64], "w_out_ab": [1, 1024, 1024], "w_in_c": [1, 1024, 1536],
    "win_q_gain": [1, 64], "win_k_gain": [1, 64], "win_sink": [1, 16], "w_out_c": [1, 1024, 1024],
    "rel_bias": [32, 16], "moe_w_group": [2, 1024, 4], "moe_b_group": [2, 4], "moe_w_router": [2, 1024, 16],
    "moe_b_router": [2, 16], "moe_w_gate": [2, 16, 1024, 512], "moe_w_up": [2, 16, 1024, 512],
    "moe_w_down": [2, 16, 512, 1024],
}


def build_program(phases=("all",), dbg=None):
    nc = bass.Bass("TRN2", target_bir_lowering=False)
    K = Ctx()
    K.nc = nc
    K.dbg = dict(dbg or {})
    K.taps = K.dbg.pop('taps', ())
    K.P = Prog()
    K.x = nc.dram_tensor("x", [S, D], F32, kind="ExternalInput").ap()
    K.used_inputs = []
    K.ident_d = nc.dram_tensor("ident", [128, 128], F32, kind="ExternalInput").ap()
    K.out = nc.dram_tensor("out", [S, D], F32, kind="ExternalOutput").ap()
    K.ropetab = nc.dram_tensor("ropetab", [S, 96], F32, kind="ExternalInput").ap()
    K.winbias = nc.dram_tensor("winbias", [16, 128, 3, 128], F32, kind="ExternalInput").ap()
    K.winmask = nc.dram_tensor("winmask", [128, 3, 128], F32, kind="ExternalInput").ap()
    skind = "ExternalOutput" if K.dbg.get("scratch_out") else "Internal"
    K.QT = nc.dram_tensor("QT", [16, 96, S], BF16, kind=skind).ap()
    K.KT = nc.dram_tensor("KT", [10, 96, S], BF16, kind=skind).ap()
    K.Vd = nc.dram_tensor("Vd", [10, 128, NT, 64], BF16, kind=skind).ap()
    K.OT = nc.dram_tensor("OT", [1024, S], BF16, kind=skind).ap()
    with ExitStack() as es:
        K.ident32 = es.enter_context(nc.sbuf_tensor("ident32", [128, 128], F32))
        K.identb = es.enter_context(nc.sbuf_tensor("identb", [128, 128], BF16))
        K.esem = {e: es.enter_context(nc.semaphore("s_" + e)) for e in ENGS}
        K.dsem = [es.enter_context(nc.semaphore("d%d" % i)) for i in range(K.P.n_dma)]
        K.block = es.enter_context(nc.Block())
        P = K.P
        P.dma("sp", lambda e: e.dma_start(out=K.ident32[:], in_=K.ident_d[:, :]), writes=["ident32"])
        P.op("dve", lambda e: e.tensor_copy(out=K.identb[:], in_=K.ident32[:]), reads=["ident32"], writes=["identb"])
        P.barrier()
        P.flush(K.block, K.esem, K.dsem)
        if "all" in phases:
            l0_prep(K, K.x)
            heads = [(h, h, 96, 96 ** -0.5, h * 64) for h in range(8)] + \
                    [(8 + j, 8 + j // 4, 64, 64 ** -0.5, 512 + j * 64) for j in range(8)]
            attn_full(K, heads)
            out_proj(K, K.w_out_ab[0], K.x, K.out)
            moe_phase(K, 0, K.out, K.out)
            l1_prep(K, K.out)
            attn_win(K)
            out_proj(K, K.w_out_c[0], K.out, K.out)
            moe_phase(K, 1, K.out, K.out)
        if "moe0" in phases:
            moe_phase(K, 0, K.x, K.out, **K.dbg)
        if "mix0" in phases:
            l0_prep(K, K.x)
            heads = [(h, h, 96, 96 ** -0.5, h * 64) for h in range(8)] + \
                    [(8 + j, 8 + j // 4, 64, 64 ** -0.5, 512 + j * 64) for j in range(8)]
            heads = heads[:K.dbg.get("nheads", 16)]
            if K.dbg.get("attn", True):
                attn_full(K, heads)
                out_proj(K, K.w_out_ab[0], K.x, K.out)
        if "mix1" in phases:
            xin = K.x if "mix0" not in phases else K.out
            l1_prep(K, xin)
            attn_win(K)
            out_proj(K, K.w_out_c[0], xin, K.out)
    nc.used_inputs = list(K.used_inputs)
    return nc


def rope_table():
    inv = (np.float32(10000.0) ** (-np.arange(0, 32, 2, dtype=np.float32) / np.float32(32))).astype(np.float32)
    pos = np.arange(S)
    tabs = []
    for p in (pos, pos // 64, pos % 64):
        ang = p.astype(np.float32)[:, None] * inv[None, :]
        tabs += [np.cos(ang), np.sin(ang)]
    return np.ascontiguousarray(np.concatenate(tabs, axis=1).astype(np.float32))


def win_tables(rel_bias):
    kk = np.arange(128)[:, None, None]
    j = np.arange(3)[None, :, None]
    qi = np.arange(128)[None, None, :]
    rel = (j * 128 + kk) - 128 - qi
    nb, max_exact = 16, 8
    ret = np.where(rel > 0, nb, 0)
    n = np.abs(rel)
    nf = np.maximum(n, 1).astype(np.float32)
    large = max_exact + (np.log(nf / np.float32(max_exact)) / np.float32(math.log(128 / max_exact))
                         * np.float32(nb - max_exact)).astype(np.int32)
    large = np.minimum(large, nb - 1)
    bucket = ret + np.where(n < max_exact, n, large)
    bias = np.ascontiguousarray(np.transpose(rel_bias[bucket], (3, 0, 1, 2))).astype(np.float32)
    mask = np.where(np.abs(rel) <= 128, 0.0, -30000.0).astype(np.float32)
    return bias, np.ascontiguousarray(mask)


_NC_CACHE = {}


def kernel(**inputs):
    n_cores = 8
    if "nc" not in _NC_CACHE:
        _NC_CACHE["nc"] = build_program(phases=("all",))
    nc = _NC_CACHE["nc"]
    x = np.ascontiguousarray(np.asarray(inputs["x"], dtype=np.float32))
    shared = {"ident": np.eye(128, dtype=np.float32), "ropetab": rope_table()}
    wb, wm = win_tables(np.asarray(inputs["rel_bias"], dtype=np.float32))
    shared["winbias"] = wb
    shared["winmask"] = wm
    for n in nc.used_inputs:
        shared[n] = np.ascontiguousarray(np.asarray(inputs[n], dtype=np.float32))
    in_maps = []
    for i in range(n_cores):
        m = dict(shared)
        m["x"] = x[i]
        in_maps.append(m)
    res = run_bass_kernel_spmd(nc, in_maps, core_ids=list(range(n_cores)))
    return np.stack([np.asarray(r["out"], dtype=np.float32) for r in res.results], axis=0)
```
